# Optimizing a Trainium2 kernel written in Bass

```python
import jax, jax.numpy as jnp
from jax import lax
import numpy as np

D_MODEL = 1024
BATCH = 8
SEQ = 2048
DEPTH = 1

D_MIX = D_MODEL
RET_WIDTH = D_MIX // 2
POOL_WIDTH = D_MIX - RET_WIDTH
RET_HEADS = 4
RET_HEAD_DIM = RET_WIDTH // RET_HEADS
RET_CHUNK = 128
ROPE_BASE = 10000.0
POOL_WINDOWS = (2, 4, 8, 16)
POOL_GROUPS = len(POOL_WINDOWS)
POOL_GROUP_DIM = POOL_WIDTH // POOL_GROUPS
IN_COLS = 4 * RET_WIDTH + POOL_WIDTH
N_GROUPS = 4
EXPERTS_PER_GROUP = 8
N_EXPERTS = N_GROUPS * EXPERTS_PER_GROUP
TOP_K_INNER = 2
D_EXPERT = 256
ALPHA = (2.0 * DEPTH) ** 0.25
BETA = (8.0 * DEPTH) ** -0.25
LN_EPS = 1e-5

kernel_name = "hymba_retention_pool_hiermoe_deepnorm_adaln"


def _layer_norm(x, w=None, b=None):
    xf = x.astype(jnp.float32)
    mu = jnp.mean(xf, axis=-1, keepdims=True)
    var = jnp.mean(jnp.square(xf - mu), axis=-1, keepdims=True)
    y = (xf - mu) * lax.rsqrt(var + LN_EPS)
    if w is not None:
        y = y * w.astype(jnp.float32) + b.astype(jnp.float32)
    return y.astype(x.dtype)


def _rotary(t, cos, sin):
    t1, t2 = jnp.split(t, 2, axis=-1)
    return jnp.concatenate([t1 * cos - t2 * sin, t1 * sin + t2 * cos], axis=-1)


def _retention(q, k, v):
    B, S, H, Dh = q.shape
    nc = S // RET_CHUNK
    lg = jnp.log1p(-jnp.exp2(-5.0 - jnp.arange(H, dtype=jnp.float32)))
    qc = q.reshape(B, nc, RET_CHUNK, H, Dh)
    kc = k.reshape(B, nc, RET_CHUNK, H, Dh)
    vc = v.reshape(B, nc, RET_CHUNK, H, Dh)
    i = jnp.arange(RET_CHUNK, dtype=jnp.float32)
    rel = i[:, None] - i[None, :]
    dmat = jnp.where(rel[None] >= 0, jnp.exp(jnp.maximum(rel, 0.0)[None] * lg[:, None, None]), 0.0)
    scores = jnp.einsum('bnchd,bnmhd->bnhcm', qc, kc) * dmat[None, None]
    intra = jnp.einsum('bnhcm,bnmhd->bnchd', scores, vc)
    k_decay = jnp.exp((RET_CHUNK - 1.0 - i)[None, :] * lg[:, None])
    kv = jnp.einsum('bnmhd,hm,bnmhe->nbhde', kc, k_decay, vc)
    chunk_decay = jnp.exp(RET_CHUNK * lg)[None, :, None, None]

    def step(state, kv_n):
        return state * chunk_decay + kv_n, state

    _, prev = lax.scan(step, jnp.zeros((B, H, Dh, Dh), jnp.float32), kv)
    q_decay = jnp.exp((i + 1.0)[:, None] * lg[None, :])
    cross = jnp.einsum('bnchd,nbhde,ch->bnche', qc, prev, q_decay)
    return (intra + cross).reshape(B, S, H, Dh)


def _multi_scale_pool(p):
    B, S, _ = p.shape
    pf = p.astype(jnp.float32)
    cs = jnp.concatenate([jnp.zeros((B, 1, POOL_WIDTH), jnp.float32), jnp.cumsum(pf, axis=1)], axis=1)
    t = jnp.arange(S)
    outs = []
    for g, w in enumerate(POOL_WINDOWS):
        lo_c, hi_c = g * POOL_GROUP_DIM, (g + 1) * POOL_GROUP_DIM
        csg = cs[:, :, lo_c:hi_c]
        upper = csg[:, 1:]
        lower = jnp.pad(csg[:, :S - w + 1], ((0, 0), (w - 1, 0), (0, 0)))
        count = jnp.minimum(t + 1, w).astype(jnp.float32)[None, :, None]
        outs.append((upper - lower) / count - pf[:, :, lo_c:hi_c])
    return jnp.stack(outs, axis=2)


def _hier_moe(u, w_group, b_group, w_router, b_router, w1, w3, w2):
    T = u.shape[0]
    gp = jax.nn.softmax((u @ w_group + b_group).astype(jnp.float32), axis=-1)
    g_prob, g_idx = lax.top_k(gp, 1)
    el = (u @ w_router + b_router).astype(jnp.float32).reshape(T, N_GROUPS, EXPERTS_PER_GROUP)
    sel = jnp.take_along_axis(el, g_idx[:, :, None], axis=1)[:, 0]
    top_v, top_i = lax.top_k(sel, TOP_K_INNER)
    cw = g_prob * jax.nn.softmax(top_v, axis=-1)
    within = jnp.einsum('tk,tke->te', cw, jax.nn.one_hot(top_i, EXPERTS_PER_GROUP, dtype=jnp.float32))
    gates = (jax.nn.one_hot(g_idx[:, 0], N_GROUPS, dtype=jnp.float32)[:, :, None] * within[:, None, :]).astype(u.dtype)
    y = jnp.zeros_like(u)
    for grp in range(N_GROUPS):
        h = jax.nn.silu(jnp.einsum('td,edf->tef', u, w1[grp])) * jnp.einsum('td,edf->tef', u, w3[grp])
        h = h * gates[:, grp, :, None]
        y = y + jnp.einsum('tef,efd->td', h, w2[grp])
    return y


def setup_inputs(seed: int = 0) -> dict:
    key = jax.random.key(seed)
    ks = jax.random.split(key, 24)
    D = D_MODEL
    nrm = jax.random.normal
    col_gain = jnp.concatenate([
        jnp.ones((2 * RET_WIDTH,), jnp.float32),
        jnp.full((RET_WIDTH,), BETA, jnp.float32),
        jnp.ones((RET_WIDTH,), jnp.float32),
        jnp.full((POOL_WIDTH,), BETA, jnp.float32),
    ])
    return {
        "x": nrm(ks[0], (BATCH, SEQ, D), jnp.float32),
        "c": nrm(ks[1], (BATCH, D), jnp.float32),
        "positions": jnp.broadcast_to(jnp.arange(SEQ, dtype=jnp.int32), (BATCH, SEQ)),
        "w_ada": nrm(ks[2], (DEPTH, D, 6 * D), jnp.float32) * D ** -0.5,
        "b_ada": 0.02 * nrm(ks[3], (DEPTH, 6 * D), jnp.float32),
        "w_in": nrm(ks[4], (DEPTH, D, IN_COLS), jnp.float32) * D ** -0.5 * col_gain,
        "ret_gn_w": 1.0 + 0.02 * nrm(ks[5], (DEPTH, RET_WIDTH), jnp.float32),
        "w_pool": nrm(ks[6], (DEPTH, POOL_GROUPS, POOL_GROUP_DIM, POOL_GROUP_DIM), jnp.float32) * POOL_GROUP_DIM ** -0.5,
        "pool_scale": 1.0 + 0.02 * nrm(ks[7], (DEPTH, POOL_WIDTH), jnp.float32),
        "w_out": nrm(ks[8], (DEPTH, D_MIX, D), jnp.float32) * D_MIX ** -0.5 * BETA,
        "ln1_w": 1.0 + 0.02 * nrm(ks[9], (DEPTH, D), jnp.float32),
        "ln1_b": 0.02 * nrm(ks[10], (DEPTH, D), jnp.float32),
        "w_group": nrm(ks[11], (DEPTH, D, N_GROUPS), jnp.float32) * D ** -0.5,
        "b_group": 0.01 * nrm(ks[12], (DEPTH, N_GROUPS), jnp.float32),
        "w_router": nrm(ks[13], (DEPTH, D, N_EXPERTS), jnp.float32) * D ** -0.5,
        "b_router": 0.01 * nrm(ks[14], (DEPTH, N_EXPERTS), jnp.float32),
        "w1": nrm(ks[15], (DEPTH, N_GROUPS, EXPERTS_PER_GROUP, D, D_EXPERT), jnp.float32) * D ** -0.5 * BETA,
        "w3": nrm(ks[16], (DEPTH, N_GROUPS, EXPERTS_PER_GROUP, D, D_EXPERT), jnp.float32) * D ** -0.5 * BETA,
        "w2": nrm(ks[17], (DEPTH, N_GROUPS, EXPERTS_PER_GROUP, D_EXPERT, D), jnp.float32) * D_EXPERT ** -0.5 * BETA,
        "ln2_w": 1.0 + 0.02 * nrm(ks[18], (DEPTH, D), jnp.float32),
        "ln2_b": 0.02 * nrm(ks[19], (DEPTH, D), jnp.float32),
    }


def reference(x, c, positions, w_ada, b_ada, w_in, ret_gn_w, w_pool, pool_scale, w_out,
              ln1_w, ln1_b, w_group, b_group, w_router, b_router, w1, w3, w2, ln2_w, ln2_b):
    B, S, D = x.shape
    inv_freq = ROPE_BASE ** (-jnp.arange(0, RET_HEAD_DIM, 2, dtype=jnp.float32) / RET_HEAD_DIM)
    ang = positions.astype(jnp.float32)[..., None] * inv_freq
    cos, sin = jnp.cos(ang)[:, :, None, :], jnp.sin(ang)[:, :, None, :]
    c_act = jax.nn.silu(c)
    for l in range(DEPTH):
        ada = (c_act @ w_ada[l] + b_ada[l])[:, None, :]
        shift1, scale1, gate1, shift2, scale2, gate2 = jnp.split(ada, 6, axis=-1)

        u = _layer_norm(x) * (1.0 + scale1) + shift1
        proj = u @ w_in[l]
        q, k, v, g, p = jnp.split(proj, [RET_WIDTH, 2 * RET_WIDTH, 3 * RET_WIDTH, 4 * RET_WIDTH], axis=-1)
        hs = (B, S, RET_HEADS, RET_HEAD_DIM)
        qr = _rotary(q.reshape(hs).astype(jnp.float32), cos, sin)
        kr = _rotary(k.reshape(hs).astype(jnp.float32), cos, sin) * RET_HEAD_DIM ** -0.5
        r = _retention(qr, kr, v.reshape(hs).astype(jnp.float32))
        r_mu = jnp.mean(r, axis=-1, keepdims=True)
        r_var = jnp.mean(jnp.square(r - r_mu), axis=-1, keepdims=True)
        r = ((r - r_mu) * lax.rsqrt(r_var + LN_EPS)).reshape(B, S, RET_WIDTH) * ret_gn_w[l]
        ret_out = jax.nn.silu(g) * r.astype(x.dtype)

        pooled = _multi_scale_pool(p).astype(x.dtype)
        pool_out = jnp.einsum('bsgc,gcd->bsgd', pooled, w_pool[l]).reshape(B, S, POOL_WIDTH) * pool_scale[l]

        mix = jnp.concatenate([ret_out, pool_out], axis=-1) @ w_out[l]
        x = _layer_norm(ALPHA * x + gate1 * mix, ln1_w[l], ln1_b[l])

        u2 = _layer_norm(x) * (1.0 + scale2) + shift2
        y = _hier_moe(u2.reshape(B * S, D), w_group[l], b_group[l], w_router[l], b_router[l],
                      w1[l], w3[l], w2[l]).reshape(B, S, D)
        x = _layer_norm(ALPHA * x + gate2 * y, ln2_w[l], ln2_b[l])
    return x
```

```python
import numpy as np
from contextlib import ExitStack
import concourse.bass as bass
import concourse.mybir as mybir
from concourse.bass_utils import run_bass_kernel_spmd

F32 = mybir.dt.float32
BF16 = mybir.dt.bfloat16
I32 = mybir.dt.int32
ALU = mybir.AluOpType
AF = mybir.ActivationFunctionType
AX = mybir.AxisListType

T = 2048
D = 1024
NT = 16
KD = 8
H = 4
NE = 32
ALPHA = 2.0 ** 0.25
LN_EPS = 1e-5
POOL_WINDOWS = (2, 4, 8, 16)
BIG = 1.0e9
TWO_PI = float(2.0 * np.pi)
EG = 2

ENGS = ("pe", "act", "dve", "pool", "sp")


class Prog:
    def __init__(self, n_dma_sems=16):
        self.ops = {e: [] for e in ENGS}
        self.cnt = {e: 0 for e in ENGS}
        self.seen = {e: {} for e in ENGS}
        self.pend = {e: {} for e in ENGS}
        self.res = {}
        self.n_dma = n_dma_sems
        self.dma_val = {}
        self.dma_rr = {e: 0 for e in ENGS}

    def _need(self, eng, waits, dep):
        if dep is None:
            return
        key, val = dep
        if key == eng and eng == "pe":
            return
        if self.seen[eng].get(key, 0) >= val:
            return
        if waits.get(key, 0) < val:
            waits[key] = val

    def _track(self, eng, waits, reads, writes, dep):
        for r in reads:
            ent = self.res.setdefault(r, {"w": None, "r": {}})
            self._need(eng, waits, ent["w"])
        for w in writes:
            ent = self.res.setdefault(w, {"w": None, "r": {}})
            self._need(eng, waits, ent["w"])
            for k, v in ent["r"].items():
                self._need(eng, waits, (k, v))
        for r in reads:
            ent = self.res[r]
            if ent["r"].get(dep[0], 0) < dep[1]:
                ent["r"][dep[0]] = dep[1]
        for w in writes:
            self.res[w] = {"w": dep, "r": {}}

    def _finish_waits(self, eng, waits):
        for k, v in self.pend[eng].items():
            if self.seen[eng].get(k, 0) < v and waits.get(k, 0) < v:
                waits[k] = v
        self.pend[eng] = {}
        for k, v in waits.items():
            self.seen[eng][k] = v
        return list(waits.items())

    def op(self, eng, fn, reads=(), writes=(), signal=True):
        if eng in ("act", "dve"):
            pr = [r for r in reads if r.startswith("pb")]
            if pr:
                reads = [r for r in reads if not r.startswith("pb")]
                writes = list(writes) + pr
        waits = {}
        dep = (eng, self.cnt[eng] + 1)
        self._track(eng, waits, reads, writes, dep)
        wl = self._finish_waits(eng, waits)
        if signal:
            self.cnt[eng] += 1
        self.ops[eng].append((wl, fn, "inc" if signal else None))

    def dma(self, eng, fn, reads=(), writes=()):
        i = self.dma_rr[eng]
        self.dma_rr[eng] = (i + 1) % self.n_dma
        key = ("d", eng, i)
        prev = self.dma_val.get(key, 0)
        waits = {}
        if prev:
            self._need(eng, waits, (key, prev))
        val = prev + 16
        self.dma_val[key] = val
        self._track(eng, waits, reads, writes, (key, val))
        wl = self._finish_waits(eng, waits)
        self.ops[eng].append((wl, fn, key))

    def barrier(self):
        cur = dict(self.cnt)
        dm = dict(self.dma_val)
        for e in ENGS:
            for k, v in cur.items():
                if k != e and v > 0 and self.pend[e].get(k, 0) < v:
                    self.pend[e][k] = v
            for k, v in dm.items():
                if self.pend[e].get(k, 0) < v:
                    self.pend[e][k] = v
        self.res = {}

    def wait_all(self, eng):
        for k, v in self.cnt.items():
            if k != eng and v > 0:
                self.pend[eng][k] = max(self.pend[eng].get(k, 0), v)
        for k, v in self.dma_val.items():
            self.pend[eng][k] = max(self.pend[eng].get(k, 0), v)

    def emit(self, nc, stack):
        sems = {}
        for e in ENGS:
            sems[e] = stack.enter_context(nc.semaphore("s_" + e))
        for key in self.dma_val:
            sems[key] = stack.enter_context(nc.semaphore("d_%s_%d" % (key[1], key[2])))
        block = stack.enter_context(nc.Block())
        ops = self.ops
        pend = self.pend

        def run(engname):
            def body(engine):
                for wl, fn, sig in ops[engname]:
                    for k, v in wl:
                        engine.wait_ge(sems[k], v)
                    ins = fn(engine)
                    if sig == "inc":
                        ins.then_inc(sems[engname], 1)
                    elif sig is not None:
                        ins.then_inc(sems[sig], 16)
                for k, v in pend[engname].items():
                    engine.wait_ge(sems[k], v)
            return body

        block.tensor(run("pe"))
        block.scalar(run("act"))
        block.vector(run("dve"))
        block.gpsimd(run("pool"))
        block.sync(run("sp"))


def _consts():
    lg = np.log1p(-np.exp2(-5.0 - np.arange(H, dtype=np.float64)))
    m = np.arange(128, dtype=np.float64)
    scale = 128.0 ** -0.5
    mask = np.zeros((128, H, 128), np.float64)
    for h in range(H):
        mask[:, h, :] = np.where(m[None, :] >= m[:, None], np.exp(-(m[:, None] + 1.0) * lg[h]) * scale, 0.0)
    qdec = np.zeros((128, H, 128), np.float64)
    for h in range(H):
        qdec[:, h, :] = np.exp((m[None, :] + 1.0) * lg[h])
    vdec = np.exp((127.0 - m[:, None]) * lg[None, :]) * scale
    cd = [float(np.exp(128.0 * lg[h])) for h in range(H)]
    inv_freq = 10000.0 ** (-np.arange(0, 128, 2, dtype=np.float64) / 128.0)
    invf = (np.concatenate([inv_freq, inv_freq]) / (2 * np.pi))[:, None]
    perm = np.zeros((128, 128), np.float64)
    for j in range(64):
        perm[j + 64, j] = -1.0
        perm[j, j + 64] = 1.0
    amat = np.zeros((128, 3, 4, 128), np.float64)
    for g, w in enumerate(POOL_WINDOWS):
        for t in range(128):
            cnt0 = min(t + 1, w)
            for s in range(max(0, t - w + 1), t + 1):
                amat[s, 0, g, t] += 1.0 / cnt0
                amat[s, 1, g, t] += 1.0 / w
            amat[t, 0, g, t] -= 1.0
            amat[t, 1, g, t] -= 1.0
            for j in range(t + 1, w):
                amat[128 + t - j, 2, g, t] += 1.0 / w
    f = lambda a: np.ascontiguousarray(a.astype(np.float32))
    return dict(ident=f(np.eye(128)), perm=f(perm), mask=f(mask.reshape(128, 512)), qdec=f(qdec.reshape(128, 512)),
                vdec=f(vdec), invf=f(invf), amat=f(amat.reshape(128, 1536))), cd


def build_nc(debug=None):
    consts, CD = _consts()
    dumps = []
    nc = bass.Bass("TRN2", target_bir_lowering=False)

    def din(name, shape, dt=F32):
        return nc.dram_tensor(name, list(shape), dt, kind="ExternalInput").ap()

    x = din("x", [T, D])
    c_col = din("c_col", [128, 8])
    pos = din("pos", [1, T], I32)
    w_ada = din("w_ada", [D, 6 * D])
    b_ada = din("b_ada", [1, 6 * D])
    w_in = din("w_in", [D, 2560])
    gnw = din("gnw", [1, 512])
    w_pool = din("w_pool", [4, 128, 128])
    pscale = din("pscale", [128, 4])
    w_out = din("w_out", [D, D])
    ln1w = din("ln1w", [1, D]); ln1b = din("ln1b", [1, D])
    ln2w = din("ln2w", [1, D]); ln2b = din("ln2b", [1, D])
    wr = din("wr", [D, 36]); br = din("br", [1, 36])
    w1 = din("w1", [NE, D, 256]); w3 = din("w3", [NE, D, 256]); w2 = din("w2", [NE, 256, D])
    c_ident = din("ident", [128, 128]); c_perm = din("perm", [128, 128])
    c_mask = din("mask", [128, 512]); c_qdec = din("qdec", [128, 512]); c_vdec = din("vdec", [128, 4])
    c_invf = din("invf", [128, 1]); c_amat = din("amat", [128, 1536])
    out = nc.dram_tensor("out", [T, D], F32, kind="ExternalOutput").ap()
    gscr = nc.dram_tensor("gscr", [32, T], BF16, kind="Internal").ap()

    P = Prog()
    st = ExitStack()
    with st:
        R0 = st.enter_context(nc.sbuf_tensor("R0", [128, 16384], F32))
        R1 = st.enter_context(nc.sbuf_tensor("R1", [128, 8192], F32))
        R2 = st.enter_context(nc.sbuf_tensor("R2", [128, 8192], F32))
        R3 = st.enter_context(nc.sbuf_tensor("R3", [128, 15360], F32))
        RC = st.enter_context(nc.sbuf_tensor("RC", [128, 1792], F32))
        pbs = [st.enter_context(nc.psum_tensor("pb%d" % i, [128, 512], F32)) for i in range(8)]

        def pb(i):
            return pbs[i][:, :]

        def pbb(i):
            return pbs[i][:, :].bitcast(BF16)

        def cv(R, off, ncol, dt=F32, pat=None, **kw):
            v = R[:, off:off + ncol]
            if dt != F32:
                v = v.bitcast(dt)
            if pat:
                v = v.rearrange(pat, **kw)
            return v

        def dump(name, ap):
            shp = list(ap.shape)
            d = nc.dram_tensor("dbg_" + name, shp, F32, kind="ExternalOutput").ap()
            P.barrier()
            P.dma("pool", lambda e: e.dma_start(out=d, in_=ap))
            dumps.append("dbg_" + name)

        def finish():
            P.barrier()
            P.wait_all("sp")
            P.emit(nc, st)
            return nc, consts

        ident = cv(RC, 0, 128)
        identb = cv(RC, 128, 64, BF16)
        permb = cv(RC, 192, 64, BF16)
        ones_f = cv(RC, 256, 128)
        adaCol = cv(RC, 384, 48)
        ccol = cv(RC, 432, 8)
        cbf = cv(RC, 440, 4, BF16)
        vdec = cv(RC, 444, 4)
        invf = cv(RC, 448, 1)
        negh = cv(RC, 449, 4)
        psc = cv(RC, 453, 4)
        one11 = cv(RC, 457, 1)
        wr_sb = cv(RC, 460, 288, F32, "p (k n) -> p k n", k=8)
        br_b = cv(RC, 748, 36)
        stat = cv(RC, 784, 64)
        rt_ = cv(RC, 848, 160)
        permf = cv(RC, 1008, 128)
        stat2 = cv(RC, 1136, 192)
        rt_alt = cv(RC, 1328, 160)
        gst_all = cv(RC, 1488, 256)
        gatesT = cv(R3, 14336, 1024, BF16)

        P.dma("sp", lambda e: e.dma_start(out=ident, in_=c_ident), writes=["ident"])
        P.dma("sp", lambda e: e.dma_start(out=permf, in_=c_perm), writes=["permf"])
        P.dma("sp", lambda e: e.dma_start(out=ccol, in_=c_col), writes=["ccol"])
        P.dma("sp", lambda e: e.dma_start(out=vdec, in_=c_vdec), writes=["vdec"])
        P.dma("sp", lambda e: e.dma_start(out=invf, in_=c_invf), writes=["invf"])
        P.dma("sp", lambda e: e.dma_start(out=psc, in_=pscale), writes=["psc"])
        P.dma("sp", lambda e: e.dma_start(out=wr_sb, in_=wr.rearrange("(k p) n -> p k n", p=128)), writes=["wr"])
        P.dma("sp", lambda e: e.dma_start(out=br_b, in_=br.partition_broadcast(128)), writes=["brb"])
        P.op("dve", lambda e: e.tensor_copy(out=identb, in_=ident), reads=["ident"], writes=["identb"])
        P.op("dve", lambda e: e.tensor_copy(out=permb, in_=permf), reads=["permf"], writes=["permb"])
        P.op("pool", lambda e: e.memset(ones_f, 1.0), writes=["ones"])
        P.op("pool", lambda e: e.memset(negh, -0.5), writes=["negh"])
        P.op("pool", lambda e: e.memset(one11, 1.0), writes=["one11"])
        P.op("act", lambda e: e.activation(out=cbf, in_=ccol, func=AF.Silu), reads=["ccol"], writes=["cbf"])

        def ln_stats(src, key, slot, want_nmr=False):
            base = slot * 24
            sb = stat2[:, base: base + 12]
            mv = stat2[:, base + 12: base + 14]
            ve = stat2[:, base + 14: base + 15]
            rs = stat2[:, base + 15: base + 16]
            nm = stat2[:, base + 16: base + 17]
            k = "stat%d" % slot
            for hh in range(2):
                P.op("dve", lambda e, hh=hh: e.bn_stats(out=sb[:, hh * 6:(hh + 1) * 6], in_=src[:, hh * 512:(hh + 1) * 512]),
                     reads=[key], writes=[k + "a"])
            P.op("dve", lambda e: e.bn_aggr(out=mv, in_=sb), reads=[k + "a"], writes=[k + "mv"])
            P.op("pool", lambda e: e.tensor_scalar_add(out=ve, in0=mv[:, 1:2], scalar1=LN_EPS), reads=[k + "mv"], writes=[k + "ve"])
            P.op("pool", lambda e: e.tensor_tensor(out=rs, in0=ve, in1=negh[:, 0:1], op=ALU.pow), reads=[k + "ve", "negh"], writes=[k + "rs"])
            if want_nmr:
                P.op("dve", lambda e: e.scalar_tensor_tensor(out=nm, in0=mv[:, 0:1], scalar=-1.0, in1=rs, op0=ALU.mult, op1=ALU.mult),
                     reads=[k + "mv", k + "rs"], writes=[k + "nm"])
                return mv[:, 0:1], rs, nm, [k + "mv", k + "rs", k + "nm"]
            return mv[:, 0:1], rs, [k + "mv", k + "rs"]

        def ln_stats_sqrt(src, key, slot):
            base = slot * 24
            sb = stat2[:, base: base + 12]
            mv = stat2[:, base + 12: base + 14]
            sd = stat2[:, base + 14: base + 15]
            rs = stat2[:, base + 15: base + 16]
            k = "stat%d" % slot
            for hh in range(2):
                P.op("dve", lambda e, hh=hh: e.bn_stats(out=sb[:, hh * 6:(hh + 1) * 6], in_=src[:, hh * 512:(hh + 1) * 512]),
                     reads=[key], writes=[k + "a"])
            P.op("dve", lambda e: e.bn_aggr(out=mv, in_=sb), reads=[k + "a"], writes=[k + "mv"])
            P.op("act", lambda e: e.activation(out=sd, in_=mv[:, 1:2], func=AF.Sqrt, bias=LN_EPS, scale=1.0), reads=[k + "mv"], writes=[k + "ve"])
            return mv[:, 0:1], sd, rs, k

        def act_stats(src, ksrc, slot, junk, kjunk, alpha=None):
            base = slot * 24
            s2_ = stat2[:, base + 0: base + 1]
            s1_ = stat2[:, base + 1: base + 2]
            ngm = stat2[:, base + 2: base + 3]
            m2_ = stat2[:, base + 3: base + 4]
            ve_ = stat2[:, base + 4: base + 5]
            rs_ = stat2[:, base + 15: base + 16]
            nb_ = stat2[:, base + 16: base + 17]
            sc_ = stat2[:, base + 17: base + 18]
            k = "as%d" % slot
            a_ = 1.0 if alpha is None else alpha
            P.op("act", lambda e: e.activation(out=junk, in_=src, func=AF.Square, accum_out=s2_), reads=[ksrc], writes=[kjunk, k + "s2"])
            if alpha is None:
                P.op("act", lambda e: e.activation(out=junk, in_=src, func=AF.Identity, accum_out=s1_), reads=[ksrc], writes=[kjunk, k + "s1"])
            else:
                P.op("act", lambda e: e.activation(out=src, in_=src, func=AF.Copy, scale=alpha, accum_out=s1_), reads=[ksrc], writes=[ksrc, k + "s1"])
            P.op("pool", lambda e: e.tensor_scalar(out=ngm, in0=s1_, scalar1=-1.0 / (1024.0 * a_), scalar2=1.0, op0=ALU.mult, op1=ALU.mult),
                 reads=[k + "s1"], writes=[k + "ngm"])
            P.op("pool", lambda e: e.tensor_tensor(out=m2_, in0=ngm, in1=ngm, op=ALU.mult), reads=[k + "ngm"], writes=[k + "m2"])
            P.op("pool", lambda e: e.tensor_scalar(out=ve_, in0=s2_, scalar1=1.0 / 1024.0, scalar2=LN_EPS, op0=ALU.mult, op1=ALU.add),
                 reads=[k + "s2"], writes=[k + "ve"])
            P.op("pool", lambda e: e.tensor_tensor(out=ve_, in0=ve_, in1=m2_, op=ALU.subtract), reads=[k + "ve", k + "m2"], writes=[k + "ve"])
            P.op("pool", lambda e: e.tensor_tensor(out=rs_, in0=ve_, in1=negh[:, 0:1], op=ALU.pow), reads=[k + "ve", "negh"], writes=[k + "rs"])
            P.op("pool", lambda e: e.tensor_tensor(out=nb_, in0=ngm, in1=rs_, op=ALU.mult), reads=[k + "ngm", k + "rs"], writes=[k + "nb"])
            if alpha is None:
                return rs_, nb_, [k + "rs", k + "nb"]
            P.op("pool", lambda e: e.tensor_scalar(out=sc_, in0=rs_, scalar1=1.0 / alpha, scalar2=1.0, op0=ALU.mult, op1=ALU.mult),
                 reads=[k + "rs"], writes=[k + "sc"])
            return sc_, nb_, [k + "sc", k + "nb"]

        uT = cv(R0, 0, 8192, BF16, "p (k t) -> p k t", k=8)
        qT = cv(R0, 8192, 4096, BF16, "p (h t) -> p h t", h=4)
        krT = cv(R0, 12288, 4096, BF16, "p (h t) -> p h t", h=4)
        k_tok = cv(R1, 0, 4096, BF16, "p (n f) -> p n f", n=16)
        v_tok = cv(R1, 4096, 4096, BF16, "p (n f) -> p n f", n=16)
        catT = cv(R2, 0, 8192, BF16, "p (k t) -> p k t", k=8)
        prev = cv(R3, 0, 4096, BF16, "p (n f) -> p n f", n=16)
        adaslot = [cv(R2, i * 2048, 2048, BF16, "p (k n) -> p k n", k=8) for i in range(4)]
        wg = cv(R3, 4096, 2048, BF16, "p (k n) -> p k n", k=8)
        TB = 6144

        arow = [cv(R1, i * 512, 512) for i in range(2)]
        brow = [cv(R1, 1024, 512), cv(R1, 1536, 512), cv(R3, 0, 512), cv(R3, 512, 512)]

        def asl(j):
            return j if j < 4 else j % 2

        def ada_load(j):
            s = asl(j)
            P.dma("pool", lambda e: e.dma_start(out=adaslot[s], in_=w_ada[:, j * 512:(j + 1) * 512].rearrange("(k p) n -> p k n", p=128)),
                  writes=["adaslot%d" % s])
            P.dma("sp", lambda e: e.dma_start(out=brow[s][0:1, :], in_=b_ada[:, j * 512:(j + 1) * 512]), writes=["brow%d" % s])

        def ada_chunk(j):
            ada_compute(j)
            if j >= 4 and j + 2 < 12:
                ada_load(j + 2)

        def ada_compute(j):
            s = j % 2
            sl_ = asl(j)
            for k in range(8):
                P.op("pe", lambda e, k=k: e.matmul(pb(7)[0:1, :], lhsT=cbf[:, k:k + 1], rhs=adaslot[sl_][:, k, :], start=(k == 0), stop=(k == 7)),
                     reads=["cbf", "adaslot%d" % sl_], writes=["pb7"], signal=(k == 7))
            P.op("dve", lambda e: e.tensor_tensor(out=arow[s][0:1, :], in0=pb(7)[0:1, :], in1=brow[sl_][0:1, :], op=ALU.add),
                 reads=["pb7", "brow%d" % sl_], writes=["arow%d" % s])
            for i in range(4):
                col = j * 4 + i
                P.op("pe", lambda e, i=i, col=col: e.matmul(pb(6)[:, col:col + 1], lhsT=arow[s][0:1, i * 128:(i + 1) * 128], rhs=one11[0:1, 0:1],
                                                          start=True, stop=True),
                     reads=["arow%d" % s, "one11"], writes=["pb6"], signal=(i == 3))
            c0, c1 = j * 4, j * 4 + 4
            addv = 1.0 if j in (2, 3, 8, 9) else 0.0
            P.op("dve", lambda e: e.tensor_scalar_add(out=adaCol[:, c0:c1], in0=pb(6)[:, c0:c1], scalar1=addv), reads=["pb6"], writes=["adaCol"])

        wslot = [cv(R3, TB + i * 2048, 2048, BF16, "p (k n) -> p k n", k=8) for i in range(2)]

        def load_win(blk, dst, key):
            P.dma("pool", lambda e: e.dma_start(out=dst, in_=w_in[:, blk * 512:(blk + 1) * 512].rearrange("(k p) n -> p k n", p=128)),
                  writes=[key])

        for j in range(4):
            ada_load(j)
        wslotA = [cv(R0, 8192 + i * 2048, 2048, BF16, "p (k n) -> p k n", k=8) for i in range(2)]

        xin = [cv(R3, TB + i * 1024, 1024) for i in range(3)]
        xn = [cv(R3, TB + 3072 + i * 512, 512, BF16) for i in range(4)]
        a1s = {}

        def a1_stats(tile):
            xb = xin[tile % 3]
            kx = "xin%d" % (tile % 3)
            P.dma("sp", lambda e: e.dma_start(out=xb, in_=x[tile * 128:(tile + 1) * 128, :]), writes=[kx])
            a1s[tile] = ln_stats_sqrt(xb, kx, tile % 2)

        def a1_norm(tile):
            i = tile % 4
            blk = tile // 4
            bo = (blk % 2) * 4
            xb = xin[tile % 3]
            kx = "xin%d" % (tile % 3)
            mean, sd_, rstd, k_ = a1s[tile]
            P.op("dve", lambda e: e.reciprocal(out=rstd, in_=sd_), reads=[k_ + "ve"], writes=[k_ + "rs"])
            P.op("dve", lambda e: e.tensor_scalar(out=xn[i], in0=xb, scalar1=mean, scalar2=rstd, op0=ALU.subtract, op1=ALU.mult),
                 reads=[kx, k_ + "mv", k_ + "rs"], writes=["xn%d" % i])
            for kc in range(8):
                P.op("pe", lambda e, kc=kc: e.transpose(out=pbb(bo + kc // 2)[:, (kc % 2) * 512 + i * 128:(kc % 2) * 512 + (i + 1) * 128],
                                                        in_=xn[i][:, kc * 128:(kc + 1) * 128], identity=identb),
                     reads=["xn%d" % i, "identb"], writes=["pb%d" % (bo + kc // 2)], signal=(kc == 7))
            if i == 3:
                if blk == 0:
                    for j in range(4):
                        ada_chunk(j)
                for kc in range(8):
                    P.op("act", lambda e, kc=kc: e.activation(out=uT[:, kc, blk * 512:(blk + 1) * 512],
                                                            in_=pbb(bo + kc // 2)[:, (kc % 2) * 512:(kc % 2 + 1) * 512],
                                                            func=AF.Identity, scale=adaCol[:, 8 + kc:9 + kc], bias=adaCol[:, kc:kc + 1]),
                         reads=["pb%d" % (bo + kc // 2), "adaCol"], writes=["uT"])
                if blk == 2:
                    load_win(2, wslotA[0], "wsA0")
                    load_win(4, wslotA[1], "wsA1")

        for t_ in range(NT + 1):
            if t_ < NT:
                a1_stats(t_)
            if t_ >= 1:
                a1_norm(t_ - 1)

        if debug == "A1":
            dump("adaCol", adaCol)
            for k in range(8):
                dump("uT%d" % k, uT[:, k, :])
            return finish()
        P.barrier()
        load_win(0, wslot[0], "ws0")
        load_win(1, wslot[1], "ws1")
        ada_load(4)
        ada_load(5)
        load_win(3, wg, "wg")
        cs_all = cv(R3, 0, 2048)
        sn_all = cv(R3, 2048, 2048)
        t_posi = cv(R1, 2048, 512, I32)
        t_posf = cv(R1, 2560, 512)
        t_rr = cv(R1, 3072, 512)
        t_nf = cv(R1, 3584, 512)
        t_ni = t_posi
        tab_thunks = []
        for tb_ in range(4):
            tbs_ = slice(tb_ * 512, (tb_ + 1) * 512)
            tab_thunks.append(lambda tbs_=tbs_: P.dma("sp", lambda e: e.dma_start(out=t_posi, in_=pos[:, tbs_].partition_broadcast(128)), writes=["posi"]))
            tab_thunks.append(lambda: P.op("dve", lambda e: e.tensor_copy(out=t_posf, in_=t_posi), reads=["posi"], writes=["posf"]))
            for dst_, key_, off_ in ((sn_all, "sn_all", 0.0), (cs_all, "cs_all", 0.25)):
                d_ = dst_[:, tbs_]
                tab_thunks.append(lambda off_=off_: P.op("dve", lambda e: e.tensor_scalar(out=t_rr, in0=t_posf, scalar1=invf[:, 0:1], scalar2=off_, op0=ALU.mult, op1=ALU.add),
                                                         reads=["posf", "invf"], writes=["rr"]))
                tab_thunks.append(lambda: P.op("dve", lambda e: e.tensor_copy(out=t_ni, in_=t_rr), reads=["rr", "posf"], writes=["posi"]))
                tab_thunks.append(lambda: P.op("dve", lambda e: e.tensor_copy(out=t_nf, in_=t_ni), reads=["posi"], writes=["nf"]))
                tab_thunks.append(lambda: P.op("dve", lambda e: e.tensor_tensor(out=t_rr, in0=t_rr, in1=t_nf, op=ALU.subtract), reads=["rr", "nf"], writes=["rr"]))
                tab_thunks.append(lambda: P.op("dve", lambda e: e.tensor_single_scalar(out=t_nf, in_=t_rr, scalar=0.5, op=ALU.is_gt), reads=["rr"], writes=["nf"]))
                tab_thunks.append(lambda: P.op("dve", lambda e: e.tensor_tensor(out=t_rr, in0=t_rr, in1=t_nf, op=ALU.subtract), reads=["rr", "nf"], writes=["rr"]))
                tab_thunks.append(lambda: P.op("dve", lambda e: e.tensor_single_scalar(out=t_nf, in_=t_rr, scalar=-0.5, op=ALU.is_lt), reads=["rr"], writes=["nf"]))
                tab_thunks.append(lambda d_=d_, key_=key_: P.op("dve", lambda e: e.tensor_tensor(out=d_, in0=t_rr, in1=t_nf, op=ALU.add), reads=["rr", "nf"], writes=[key_]))
        p_tok = [cv(R3, TB + 4096 + i * 512, 512) for i in range(2)]
        pooledT = cv(R3, TB + 5120, 1024, BF16, "p (g t) -> p g t", g=4)
        amat = cv(R3, TB + 6144, 1536, F32, "p (k g t) -> p k g t", k=3, g=4)
        wpool = cv(R3, TB + 7680, 256, BF16, "p (g d) -> p g d", g=4)
        P.dma("sp", lambda e: e.dma_start(out=amat, in_=c_amat.rearrange("p (k g t) -> p k g t", k=3, g=4)), writes=["amat"])
        P.dma("pool", lambda e: e.dma_start(out=wpool, in_=w_pool.rearrange("g c d -> c g d")), writes=["wpool"])
        for tile in range(NT):
            ts = slice(tile * 128, (tile + 1) * 128)
            bv = tile % 2
            for kc in range(8):
                P.op("pe", lambda e, kc=kc, ts=ts, bv=bv: e.matmul(pb(bv), lhsT=uT[:, kc, ts], rhs=wslotA[0][:, kc, :], start=(kc == 0), stop=(kc == 7)),
                     reads=["uT", "wsA0"], writes=["pb%d" % bv], signal=(kc == 7))
            P.op("act", lambda e, tile=tile, bv=bv: e.activation(out=v_tok[:, tile, :], in_=pb(bv), func=AF.Copy), reads=["pb%d" % bv], writes=["v_tok"])
            bp = 2 + tile % 2
            for kc in range(8):
                P.op("pe", lambda e, kc=kc, ts=ts, bp=bp: e.matmul(pb(bp), lhsT=uT[:, kc, ts], rhs=wslotA[1][:, kc, :], start=(kc == 0), stop=(kc == 7)),
                     reads=["uT", "wsA1"], writes=["pb%d" % bp], signal=(kc == 7))
            pc = p_tok[tile % 2]
            pp = p_tok[(tile - 1) % 2]
            P.op("act", lambda e, pc=pc, bp=bp: e.activation(out=pc, in_=pb(bp), func=AF.Copy), reads=["pb%d" % bp], writes=["ptok%d" % (tile % 2)])
            bq = 4 + tile % 2
            for g in range(4):
                gs = slice(g * 128, (g + 1) * 128)
                kind = 0 if tile == 0 else 1
                P.op("pe", lambda e, g=g, gs=gs, pc=pc, bq=bq, kind=kind, tile=tile: e.matmul(pb(bq)[:, gs], lhsT=pc[:, gs], rhs=amat[:, kind, g, :],
                                                                                           start=True, stop=(tile == 0)),
                     reads=["ptok%d" % (tile % 2), "amat"], writes=["pb%d" % bq], signal=(tile == 0 and g == 3))
                if tile > 0:
                    P.op("pe", lambda e, g=g, gs=gs, pp=pp, bq=bq: e.matmul(pb(bq)[:, gs], lhsT=pp[:, gs], rhs=amat[:, 2, g, :], start=False, stop=True),
                         reads=["ptok%d" % ((tile - 1) % 2), "amat"], writes=["pb%d" % bq], signal=(g == 3))
            for _ in range(5):
                if tab_thunks:
                    tab_thunks.pop(0)()
            ti = tile % 4
            P.op("act", lambda e, bq=bq, ti=ti: e.activation(out=pooledT[:, :, ti * 128:(ti + 1) * 128],
                                                            in_=pb(bq).rearrange("p (g t) -> p g t", g=4), func=AF.Copy),
                 reads=["pb%d" % bq], writes=["pooledT"])
            if ti == 3:
                ada_chunk(4 + tile // 4)
            if ti == 3:
                blk = tile // 4
                for g in range(4):
                    bw = 6 + g % 2
                    P.op("pe", lambda e, g=g, bw=bw: e.matmul(pb(bw), lhsT=wpool[:, g, :], rhs=pooledT[:, g, :], start=True, stop=True),
                         reads=["wpool", "pooledT"], writes=["pb%d" % bw])
                    P.op("act", lambda e, g=g, bw=bw, blk=blk: e.activation(out=catT[:, 4 + g, blk * 512:(blk + 1) * 512], in_=pb(bw),
                                                                          func=AF.Identity, scale=psc[:, g:g + 1]),
                         reads=["pb%d" % bw, "psc"], writes=["catT"])

        while tab_thunks:
            tab_thunks.pop(0)()
        P.op("act", lambda e: e.activation(out=sn_all, in_=sn_all, func=AF.Sin, scale=TWO_PI), reads=["sn_all"], writes=["sn_all"])
        P.op("act", lambda e: e.activation(out=cs_all, in_=cs_all, func=AF.Sin, scale=TWO_PI), reads=["cs_all"], writes=["cs_all"])
        if debug == "A2a":
            for n in range(0, 16, 5):
                dump("v%d" % n, v_tok[:, n, :])
            for k in range(4, 8):
                dump("catT%d" % k, catT[:, k, :])
            return finish()
        P.barrier()
        qdec = cv(R3, 14080, 512, F32, "p (h c) -> p h c", h=4)
        qb = [cv(R3, TB + 4096 + i * 256, 256, BF16) for i in range(2)]
        rt1 = [cv(R3, TB + 4608 + i * 512, 512) for i in range(2)]
        rt2 = [cv(R3, TB + 5632 + i * 512, 512) for i in range(2)]
        P.dma("sp", lambda e: e.dma_start(out=qdec, in_=c_qdec.rearrange("p (h c) -> p h c", h=4)), writes=["qdec"])
        units = [(tb, which, h) for which in range(2) for tb in range(4) for h in range(4)]

        def u_s0(u):
            tb, which, h = units[u]
            tbs = slice(tb * 512, (tb + 1) * 512)
            hs = slice(h * 128, (h + 1) * 128)
            ba = u % 2
            cb = u % 2
            for kc in range(8):
                P.op("pe", lambda e, kc=kc: e.matmul(pb(ba), lhsT=wslot[which][:, kc, hs], rhs=uT[:, kc, tbs], start=(kc == 0), stop=(kc == 7)),
                     reads=["ws%d" % which, "uT"], writes=["pb%d" % ba], signal=(kc == 7))
            P.op("act", lambda e: e.activation(out=qb[cb], in_=pb(ba), func=AF.Copy), reads=["pb%d" % ba], writes=["qb%d" % cb])

        def u_s1(u):
            tb, which, h = units[u]
            tbs = slice(tb * 512, (tb + 1) * 512)
            tp_ = tb % 2
            ba = u % 2
            bb = 2 + u % 2
            cb = u % 2
            P.op("pe", lambda e: e.matmul(pb(bb), lhsT=permb, rhs=qb[cb], start=True, stop=True), reads=["permb", "qb%d" % cb], writes=["pb%d" % bb])
            P.op("dve", lambda e: e.tensor_tensor(out=rt1[cb], in0=pb(ba), in1=cs_all[:, tbs], op=ALU.mult),
                 reads=["pb%d" % ba, "qb%d" % cb], writes=["rt1%d" % cb])
            P.op("dve", lambda e: e.tensor_tensor(out=rt2[cb], in0=pb(bb), in1=sn_all[:, tbs], op=ALU.mult),
                 reads=["pb%d" % bb], writes=["rt2%d" % cb])
            if which == 0:
                P.op("pool", lambda e: e.tensor_tensor(out=rt1[cb], in0=rt1[cb], in1=rt2[cb], op=ALU.add),
                     reads=["rt1%d" % cb, "rt2%d" % cb], writes=["rt1%d" % cb])
                P.op("dve", lambda e: e.tensor_tensor(out=qT[:, h, tbs].rearrange("p (n c) -> p n c", n=4),
                                                       in0=rt1[cb].rearrange("p (n c) -> p n c", n=4),
                                                       in1=qdec[:, h, :].unsqueeze(1).to_broadcast([128, 4, 128]), op=ALU.mult),
                     reads=["rt1%d" % cb, "qdec"], writes=["qT"])
            else:
                P.op("pool", lambda e: e.tensor_tensor(out=krT[:, h, tbs], in0=rt1[cb], in1=rt2[cb], op=ALU.add),
                     reads=["rt1%d" % cb, "rt2%d" % cb], writes=["krT"])

        for u in range(len(units) + 1):
            if u < len(units):
                tb, which, h = units[u]
                if which == 0 and h == 0:
                    ada_chunk(8 + tb)
                u_s0(u)
            if u >= 1:
                u_s1(u - 1)

        if debug == "A2b":
            for h in range(4):
                dump("qT%d" % h, qT[:, h, :])
                dump("krT%d" % h, krT[:, h, :])
            return finish()
        P.barrier()
        S = cv(R3, TB, 512)
        maskT = cv(R3, TB + 512, 512, F32, "p (h c) -> p h c", h=4)
        gnw_b = cv(R3, TB + 1024, 512)
        sT = [cv(R3, TB + 1536 + i * 256, 256, BF16, "p (h c) -> p h c", h=4) for i in range(2)]
        rn = [cv(R3, TB + 2048 + i * 512, 512) for i in range(2)]
        sg = [cv(R3, TB + 3072 + i * 256, 256, BF16) for i in range(6)]
        ro = [cv(R3, TB + 4608 + i * 256, 256, BF16) for i in range(2)]
        r_sb = [cv(R3, TB + 5120 + i * 512, 512) for i in range(4)]
        P.dma("sp", lambda e: e.dma_start(out=maskT, in_=c_mask.rearrange("p (h c) -> p h c", h=4)), writes=["maskT"])
        P.dma("sp", lambda e: e.dma_start(out=gnw_b, in_=gnw.partition_broadcast(128)), writes=["gnwb"])
        for tile in range(NT):
            ts = slice(tile * 128, (tile + 1) * 128)
            bk = tile % 2
            for h in range(4):
                P.op("pe", lambda e, h=h, ts=ts, bk=bk: e.transpose(out=pbb(bk)[:, h * 128:(h + 1) * 128], in_=krT[:, h, ts], identity=identb),
                     reads=["krT", "identb"], writes=["pb%d" % bk], signal=(h == 3))
            P.op("dve", lambda e, tile=tile, bk=bk: e.tensor_tensor(out=k_tok[:, tile, :].rearrange("p (h d) -> p h d", h=4),
                                                                  in0=pbb(bk)[:, 0:512].rearrange("p (h d) -> p h d", h=4),
                                                                  in1=vdec.unsqueeze(2).to_broadcast([128, 4, 128]), op=ALU.mult),
                 reads=["pb%d" % bk, "vdec"], writes=["k_tok"])
        P.op("pool", lambda e: e.memset(S, 0.0), writes=["S"])
        def state_step(n):
            P.op("act", lambda e: e.activation(out=prev[:, n, :], in_=S, func=AF.Copy), reads=["S"], writes=["prev%d" % n])
            if n < NT - 1:
                bs = 2 + n % 2
                for h in range(4):
                    hs = slice(h * 128, (h + 1) * 128)
                    P.op("pe", lambda e, hs=hs: e.matmul(pb(bs)[:, hs], lhsT=k_tok[:, n, hs], rhs=v_tok[:, n, hs], start=True, stop=True),
                         reads=["k_tok", "v_tok"], writes=["pb%d" % bs], signal=(h == 3))
                for h in range(4):
                    hs = slice(h * 128, (h + 1) * 128)
                    P.op("dve", lambda e, h=h, hs=hs: e.scalar_tensor_tensor(out=S[:, hs], in0=S[:, hs], scalar=CD[h], in1=pb(bs)[:, hs],
                                                                         op0=ALU.mult, op1=ALU.add),
                         reads=["S", "pb%d" % bs], writes=["S"])

        state_step(0)

        def gset(n):
            base = (n % 4) * 64
            return (gst_all[:, base:base + 24], gst_all[:, base + 24:base + 32], gst_all[:, base + 32:base + 36], gst_all[:, base + 36:base + 40], "gs%d" % (n % 4))

        def a4_sc(n):
            ts = slice(n * 128, (n + 1) * 128)
            ba = 4 + n % 2
            for h in range(4):
                hs = slice(h * 128, (h + 1) * 128)
                P.op("pe", lambda e, h=h, hs=hs: e.matmul(pb(ba)[:, hs], lhsT=krT[:, h, ts], rhs=qT[:, h, ts], start=True, stop=True),
                     reads=["krT", "qT"], writes=["pb%d" % ba], signal=(h == 3))

        def a4_mask(n):
            ba = 4 + n % 2
            P.op("dve", lambda e: e.tensor_tensor(out=sT[n % 2], in0=pb(ba).rearrange("p (h c) -> p h c", h=4), in1=maskT, op=ALU.mult),
                 reads=["pb%d" % ba, "maskT"], writes=["sT%d" % (n % 2)])

        def a4_rg(n):
            ts = slice(n * 128, (n + 1) * 128)
            br_ = 6 + n % 2
            bg = n % 2
            for h in range(4):
                hs = slice(h * 128, (h + 1) * 128)
                P.op("pe", lambda e, h=h, hs=hs: e.matmul(pb(br_)[:, hs], lhsT=sT[n % 2][:, h, :], rhs=v_tok[:, n, hs], start=True, stop=False),
                     reads=["sT%d" % (n % 2), "v_tok"], writes=["pb%d" % br_], signal=False)
                P.op("pe", lambda e, h=h, hs=hs: e.matmul(pb(br_)[:, hs], lhsT=qT[:, h, ts], rhs=prev[:, n, hs], start=False, stop=True),
                     reads=["qT", "prev%d" % n], writes=["pb%d" % br_], signal=(h == 3))
            for kc in range(8):
                P.op("pe", lambda e, kc=kc: e.matmul(pb(bg), lhsT=uT[:, kc, ts], rhs=wg[:, kc, :], start=(kc == 0), stop=(kc == 7)),
                     reads=["uT", "wg"], writes=["pb%d" % bg], signal=(kc == 7))

        def a4_ev(n):
            br_ = 6 + n % 2
            bg = n % 2
            P.op("act", lambda e: e.activation(out=r_sb[n % 4], in_=pb(br_), func=AF.Copy), reads=["pb%d" % br_], writes=["rsb%d" % (n % 4)])
            P.op("act", lambda e: e.activation(out=sg[n % 6], in_=pb(bg), func=AF.Silu), reads=["pb%d" % bg], writes=["sg%d" % (n % 6)])

        def a4_st(n):
            gst, gmv, gve, grs, gk = gset(n)
            for h in range(4):
                hs = slice(h * 128, (h + 1) * 128)
                P.op("dve", lambda e, h=h, hs=hs: e.bn_stats(out=gst[:, h * 6:(h + 1) * 6], in_=r_sb[n % 4][:, hs]),
                     reads=["rsb%d" % (n % 4)], writes=[gk + "st"])
            for h in range(4):
                P.op("dve", lambda e, h=h: e.bn_aggr(out=gmv[:, h * 2:(h + 1) * 2], in_=gst[:, h * 6:(h + 1) * 6]), reads=[gk + "st"], writes=[gk + "mv"])

        def a4_rs(n):
            gst, gmv, gve, grs, gk = gset(n)
            P.op("pool", lambda e: e.tensor_scalar_add(out=gve, in0=gmv.rearrange("p (h two) -> p h two", two=2)[:, :, 1], scalar1=LN_EPS),
                 reads=[gk + "mv"], writes=[gk + "ve"])
            P.op("pool", lambda e: e.tensor_tensor(out=grs, in0=gve, in1=negh, op=ALU.pow), reads=[gk + "ve", "negh"], writes=[gk + "rs"])

        def a4_nm(n):
            gst, gmv, gve, grs, gk = gset(n)
            for h in range(4):
                hs = slice(h * 128, (h + 1) * 128)
                P.op("dve", lambda e, h=h, hs=hs: e.tensor_scalar(out=rn[n % 2][:, hs], in0=r_sb[n % 4][:, hs], scalar1=gmv[:, 2 * h:2 * h + 1],
                                                              scalar2=grs[:, h:h + 1], op0=ALU.subtract, op1=ALU.mult),
                     reads=["rsb%d" % (n % 4), gk + "mv", gk + "rs"], writes=["rn%d" % (n % 2)])

        def a4_gt(n):
            P.op("pool", lambda e: e.tensor_tensor(out=rn[n % 2], in0=rn[n % 2], in1=gnw_b, op=ALU.mult),
                 reads=["rn%d" % (n % 2), "gnwb"], writes=["rn%d" % (n % 2)])
            P.op("pool", lambda e: e.tensor_tensor(out=ro[n % 2], in0=rn[n % 2], in1=sg[n % 6], op=ALU.mult),
                 reads=["rn%d" % (n % 2), "sg%d" % (n % 6)], writes=["ro%d" % (n % 2)])

        def a4_tr(n):
            bt = 2 + n % 2
            for h in range(4):
                hs = slice(h * 128, (h + 1) * 128)
                P.op("pe", lambda e, h=h, hs=hs: e.transpose(out=pbb(bt)[:, hs], in_=ro[n % 2][:, hs], identity=identb),
                     reads=["ro%d" % (n % 2), "identb"], writes=["pb%d" % bt], signal=(h == 3))

        def a4_ct(n):
            ts = slice(n * 128, (n + 1) * 128)
            bt = 2 + n % 2
            P.op("act", lambda e: e.activation(out=catT[:, 0:4, ts], in_=pbb(bt)[:, 0:512].rearrange("p (h t) -> p h t", h=4), func=AF.Copy),
                 reads=["pb%d" % bt], writes=["catT"])

        a4_stages = [a4_sc, a4_mask, a4_rg, a4_ev, a4_st, a4_rs, a4_nm, a4_gt, a4_tr, a4_ct]
        for step in range(NT + len(a4_stages) - 1):
            for j in range(len(a4_stages) - 1, -1, -1):
                t_ = step - j
                if 0 <= t_ < NT:
                    a4_stages[j](t_)
            if step + 1 < NT:
                state_step(step + 1)

        if debug == "A4":
            for n in (0, 1, 15):
                dump("prev%d" % n, prev[:, n, :])
                dump("ktok%d" % n, k_tok[:, n, :])
            for k in range(4):
                dump("catT%d" % k, catT[:, k, :])
            return finish()
        P.barrier()
        acc = cv(R0, 0, 16384, F32, "p (n d) -> p n d", n=16)
        u2T = cv(R1, 0, 8192, BF16, "p (k t) -> p k t", k=8)
        woutg = cv(R3, 0, 4096, BF16, "p (k d) -> p k d", k=8)
        ln1w_b = cv(R3, 4096, 1024)
        ln1b_b = cv(R3, 5120, 1024)
        gate1_b = cv(R3, 6144, 1024)
        xin2 = [cv(R3, 7168 + i * 1024, 1024) for i in range(2)]
        xn2 = cv(R3, 9216, 1024)
        u2f = cv(R3, 10240, 1024, F32, "p (k t) -> p k t", k=8)
        dg = cv(R3, 11264, 256)
        P.dma("pool", lambda e: e.dma_start(out=woutg, in_=w_out.rearrange("(k p) d -> p k d", p=128)), writes=["woutg"])
        P.dma("sp", lambda e: e.dma_start(out=ln1w_b, in_=ln1w.partition_broadcast(128)), writes=["ln1w"])
        P.dma("sp", lambda e: e.dma_start(out=ln1b_b, in_=ln1b.partition_broadcast(128)), writes=["ln1b"])

        def build_gate_b(col0, gdst):
            for kc in range(8):
                d_ = dg[:, (kc % 2) * 128:(kc % 2 + 1) * 128]
                P.op("dve", lambda e, kc=kc, d_=d_: e.tensor_scalar(out=d_, in0=ident, scalar1=adaCol[:, col0 + kc:col0 + kc + 1], scalar2=None, op0=ALU.mult),
                     reads=["ident", "adaCol"], writes=["dg%d" % (kc % 2)])
                P.op("pe", lambda e, kc=kc, d_=d_: e.matmul(pb(kc // 4)[:, (kc % 4) * 128:(kc % 4 + 1) * 128], lhsT=ones_f, rhs=d_, start=True, stop=True),
                     reads=["ones", "dg%d" % (kc % 2)], writes=["pb%d" % (kc // 4)])
            for hh in range(2):
                P.op("act", lambda e, hh=hh: e.activation(out=gdst[:, hh * 512:(hh + 1) * 512], in_=pb(hh), func=AF.Copy),
                     reads=["pb%d" % hh], writes=["gate_b"])

        build_gate_b(16, gate1_b)
        P.op("dve", lambda e: e.tensor_tensor(out=woutg[:, 0:5, :], in0=woutg[:, 0:5, :], in1=gate1_b.unsqueeze(1).to_broadcast([128, 5, 1024]), op=ALU.mult),
             reads=["woutg", "gate_b"], writes=["woutgA"])
        P.op("pool", lambda e: e.tensor_tensor(out=woutg[:, 5:8, :], in0=woutg[:, 5:8, :], in1=gate1_b.unsqueeze(1).to_broadcast([128, 3, 1024]), op=ALU.mult),
             reads=["woutg", "gate_b"], writes=["woutgB"])
        xn2b = [xn2, cv(R3, 12288, 1024)]
        u2fb = [u2f, cv(R3, 13312, 1024, F32, "p (k t) -> p k t", k=8)]
        lgt_all = cv(R3, 11584, 576, F32, "p (n c) -> p n c", n=16)
        st2 = {}

        def a5_mm(tile):
            pr = tile % 2
            ts = slice(tile * 128, (tile + 1) * 128)
            xb = xin2[pr]; kx = "xin2%d" % pr
            P.dma("sp", lambda e: e.dma_start(out=xb, in_=x[ts, :]), writes=[kx])
            for hh in range(2):
                bm = pr * 2 + hh
                for kc in range(8):
                    P.op("pe", lambda e, kc=kc, bm=bm, hh=hh: e.matmul(pb(bm), lhsT=catT[:, kc, ts], rhs=woutg[:, kc, hh * 512:(hh + 1) * 512],
                                                                    start=(kc == 0), stop=(kc == 7)),
                         reads=["catT", "woutg", "woutgA", "woutgB"], writes=["pb%d" % bm], signal=(kc == 7))

        def a5_res(tile):
            pr = tile % 2
            xb = xin2[pr]; kx = "xin2%d" % pr
            at = acc[:, tile, :]; ka = "acc%d" % tile
            for hh in range(2):
                bm = pr * 2 + hh
                P.op("dve", lambda e, bm=bm, hh=hh: e.scalar_tensor_tensor(out=at[:, hh * 512:(hh + 1) * 512], in0=xb[:, hh * 512:(hh + 1) * 512],
                                                                        scalar=ALPHA, in1=pb(bm), op0=ALU.mult, op1=ALU.add),
                     reads=[kx, "pb%d" % bm], writes=[ka])
            st2[("ln1", tile)] = ln_stats(at, ka, pr)

        def a5_ln1(tile):
            pr = tile % 2
            at = acc[:, tile, :]; ka = "acc%d" % tile
            mean, rstd, sk = st2[("ln1", tile)]
            P.op("dve", lambda e: e.scalar_tensor_tensor(out=at, in0=at, scalar=mean, in1=ln1w_b, op0=ALU.subtract, op1=ALU.mult),
                 reads=[ka, "ln1w"] + sk, writes=[ka])
            P.op("dve", lambda e: e.scalar_tensor_tensor(out=at, in0=at, scalar=rstd, in1=ln1b_b, op0=ALU.mult, op1=ALU.add),
                 reads=[ka, "ln1b"] + sk, writes=[ka])

        def a5_sq(tile):
            pr = tile % 2
            at = acc[:, tile, :]; ka = "acc%d" % tile
            st2[("ln2", tile)] = act_stats(at, ka, 2 + pr, gate1_b, "gate_b", alpha=ALPHA)

        def a5_xn(tile):
            pr = tile % 2
            at = acc[:, tile, :]; ka = "acc%d" % tile
            xq = xn2b[pr]; kxn = "xn2%d" % pr
            scl, nb, sk2 = st2[("ln2", tile)]
            P.op("act", lambda e: e.activation(out=xq, in_=at, func=AF.Identity, scale=scl, bias=nb), reads=[ka] + sk2, writes=[kxn])

        def a5_tr(tile):
            pr = tile % 2
            xq = xn2b[pr]; kxn = "xn2%d" % pr
            for kc in range(8):
                bu = 4 + kc // 4
                P.op("pe", lambda e, kc=kc, bu=bu: e.transpose(out=pb(bu)[:, (kc % 4) * 128:(kc % 4 + 1) * 128], in_=xq[:, kc * 128:(kc + 1) * 128], identity=ident),
                     reads=[kxn, "ident"], writes=["pb%d" % bu], signal=(kc % 4 == 3))

        def a5_ev(tile):
            pr = tile % 2
            uf = u2fb[pr]; kuf = "u2f%d" % pr
            for kc in range(8):
                bu = 4 + kc // 4
                P.op("act", lambda e, kc=kc, bu=bu: e.activation(out=uf[:, kc, :], in_=pb(bu)[:, (kc % 4) * 128:(kc % 4 + 1) * 128], func=AF.Identity,
                                                               scale=adaCol[:, 32 + kc:33 + kc], bias=adaCol[:, 24 + kc:25 + kc]),
                     reads=["pb%d" % bu, "adaCol"], writes=[kuf])

        def a5_rt(tile):
            pr = tile % 2
            ts = slice(tile * 128, (tile + 1) * 128)
            uf = u2fb[pr]; kuf = "u2f%d" % pr
            P.op("pool", lambda e: e.tensor_copy(out=u2T[:, :, ts], in_=uf), reads=[kuf], writes=["u2T"])
            bl = 6 + pr
            for kc in range(8):
                P.op("pe", lambda e, kc=kc: e.matmul(pb(bl)[:, 0:36], lhsT=uf[:, kc, :], rhs=wr_sb[:, kc, :], start=(kc == 0), stop=(kc == 7)),
                     reads=[kuf, "wr"], writes=["pb%d" % bl], signal=(kc == 7))

        def a5_lg(tile):
            bl = 6 + tile % 2
            P.op("dve", lambda e: e.tensor_tensor(out=lgt_all[:, tile, :], in0=pb(bl)[:, 0:36], in1=br_b, op=ALU.add),
                 reads=["pb%d" % bl, "brb"], writes=["lgt"])

        a5_stages = [a5_mm, a5_res, a5_ln1, a5_sq, a5_xn, a5_tr, a5_ev, a5_rt, a5_lg]
        w13 = [cv(R2, i * 2048, 2048, BF16, "p (a k f) -> p a k f", a=2, k=8) for i in range(4)]
        w2s = [cv(R3, i * 1024, 1024, BF16, "p (c d) -> p c d", c=2) for i in range(4)]

        loadq = []

        def load_expert(ex, slot, extra=()):
            ex_ = list(extra)
            loadq.append(lambda: P.dma("pool", lambda e: e.dma_start(out=w13[slot][:, 0], in_=w1[ex].rearrange("(k p) f -> p k f", p=128)),
                                       writes=["w13_%d" % slot] + ex_))
            loadq.append(lambda: P.dma("pool", lambda e: e.dma_start(out=w13[slot][:, 1], in_=w3[ex].rearrange("(k p) f -> p k f", p=128)),
                                       writes=["w13_%d" % slot] + ex_))
            loadq.append(lambda: P.dma("pool", lambda e: e.dma_start(out=w2s[slot], in_=w2[ex].rearrange("(c p) d -> p c d", p=128)),
                                       writes=["w2_%d" % slot] + ex_))

        def pump(n):
            for _ in range(n):
                if loadq:
                    loadq.pop(0)()

        for step in range(NT + len(a5_stages) - 1):
            for j in range(len(a5_stages) - 1, -1, -1):
                t_ = step - j
                if 0 <= t_ < NT:
                    a5_stages[j](t_)
            if step == NT:
                for i in range(EG):
                    load_expert(i, i, extra=["catT", "woutg", "woutgA", "woutgB"])
            if step >= NT:
                pump(1)

        pump(len(loadq))
        RB = 7168
        def rc(off, ncol):
            return cv(R3, RB + off, ncol)
        gmax = rc(0, 16); gmask = rc(16, 64); sh4 = rc(80, 64); gsum = rc(144, 16); gprob = rc(160, 16); pen = rc(176, 64)
        v1 = rc(240, 16); v2 = rc(256, 16); dd = rc(272, 16); ed = rc(288, 16); p1 = rc(304, 16); p2 = rc(320, 16)
        elm = rc(512, 512); m1 = rc(1024, 512); m2 = rc(1536, 512); gts_all = rc(2048, 512); tmpg = rc(2560, 512)
        K_ = "rtb"
        L4 = lgt_all[:, :, 32:36]
        g3 = lambda ap_, w: ap_.rearrange("p (n c) -> p n c", n=16) if w else ap_
        P.op("dve", lambda e: e.tensor_reduce(out=gmax, in_=L4, axis=AX.X, op=ALU.max), reads=["lgt"], writes=[K_, "xin20", "xin21", "xn20"])
        P.op("dve", lambda e: e.tensor_tensor(out=g3(gmask, 1), in0=L4, in1=gmax.unsqueeze(2).to_broadcast([128, 16, 4]), op=ALU.is_ge), reads=["lgt", K_], writes=[K_])
        P.op("dve", lambda e: e.tensor_tensor(out=g3(sh4, 1), in0=L4, in1=gmax.unsqueeze(2).to_broadcast([128, 16, 4]), op=ALU.subtract), reads=["lgt", K_], writes=[K_])
        P.op("act", lambda e: e.activation(out=sh4, in_=sh4, func=AF.Exp), reads=[K_], writes=[K_])
        P.op("dve", lambda e: e.tensor_reduce(out=gsum, in_=g3(sh4, 1), axis=AX.X, op=ALU.add), reads=[K_], writes=[K_])
        P.op("dve", lambda e: e.reciprocal(out=gprob, in_=gsum), reads=[K_], writes=[K_])
        P.op("dve", lambda e: e.tensor_scalar(out=pen, in0=gmask, scalar1=1.0, scalar2=BIG, op0=ALU.subtract, op1=ALU.mult), reads=[K_], writes=[K_])
        P.op("dve", lambda e: e.tensor_tensor(out=elm.rearrange("p (n g i) -> p n g i", n=16, g=4),
                                              in0=lgt_all[:, :, 0:32].rearrange("p n (g i) -> p n g i", g=4),
                                              in1=g3(pen, 1).unsqueeze(3).to_broadcast([128, 16, 4, 8]), op=ALU.add), reads=["lgt", K_], writes=[K_])
        P.op("dve", lambda e: e.tensor_reduce(out=v1, in_=g3(elm, 1), axis=AX.X, op=ALU.max), reads=[K_], writes=[K_])
        P.op("dve", lambda e: e.tensor_tensor(out=g3(m1, 1), in0=g3(elm, 1), in1=v1.unsqueeze(2).to_broadcast([128, 16, 32]), op=ALU.is_ge), reads=[K_], writes=[K_])
        P.op("dve", lambda e: e.scalar_tensor_tensor(out=elm, in0=m1, scalar=-BIG, in1=elm, op0=ALU.mult, op1=ALU.add), reads=[K_], writes=[K_])
        P.op("dve", lambda e: e.tensor_reduce(out=v2, in_=g3(elm, 1), axis=AX.X, op=ALU.max), reads=[K_], writes=[K_])
        P.op("dve", lambda e: e.tensor_tensor(out=g3(m2, 1), in0=g3(elm, 1), in1=v2.unsqueeze(2).to_broadcast([128, 16, 32]), op=ALU.is_ge), reads=[K_], writes=[K_])
        P.op("dve", lambda e: e.tensor_tensor(out=dd, in0=v2, in1=v1, op=ALU.subtract), reads=[K_], writes=[K_])
        P.op("act", lambda e: e.activation(out=ed, in_=dd, func=AF.Exp), reads=[K_], writes=[K_])
        P.op("dve", lambda e: e.tensor_scalar_add(out=p1, in0=ed, scalar1=1.0), reads=[K_], writes=[K_])
        P.op("dve", lambda e: e.reciprocal(out=p1, in_=p1), reads=[K_], writes=[K_])
        P.op("dve", lambda e: e.tensor_tensor(out=p2, in0=ed, in1=p1, op=ALU.mult), reads=[K_], writes=[K_])
        P.op("dve", lambda e: e.tensor_tensor(out=p1, in0=p1, in1=gprob, op=ALU.mult), reads=[K_], writes=[K_])
        P.op("dve", lambda e: e.tensor_tensor(out=p2, in0=p2, in1=gprob, op=ALU.mult), reads=[K_], writes=[K_])
        P.op("dve", lambda e: e.tensor_tensor(out=g3(gts_all, 1), in0=g3(m1, 1), in1=p1.unsqueeze(2).to_broadcast([128, 16, 32]), op=ALU.mult), reads=[K_], writes=[K_])
        P.op("dve", lambda e: e.tensor_tensor(out=g3(tmpg, 1), in0=g3(m2, 1), in1=p2.unsqueeze(2).to_broadcast([128, 16, 32]), op=ALU.mult), reads=[K_], writes=[K_])
        P.op("dve", lambda e: e.tensor_tensor(out=gts_all, in0=gts_all, in1=tmpg, op=ALU.add), reads=[K_], writes=[K_])
        for q4 in range(4):
            for i in range(4):
                n = q4 * 4 + i
                P.op("pe", lambda e, n=n, q4=q4, i=i: e.transpose(out=pb(q4)[0:32, i * 128:(i + 1) * 128], in_=gts_all[:, n * 32:(n + 1) * 32], identity=ident),
                     reads=[K_, "ident"], writes=["pb%d" % q4], signal=(i == 3))
            P.op("act", lambda e, q4=q4: e.activation(out=gatesT[0:32, q4 * 512:(q4 + 1) * 512], in_=pb(q4)[0:32, :], func=AF.Copy),
                 reads=["pb%d" % q4], writes=["gatesT"])
        P.dma("sp", lambda e: e.dma_start(out=gscr, in_=gatesT[0:32, :]), reads=["gatesT"], writes=["gscr"])

        if debug == "A5":
            for n in (0, 7, 15):
                dump("acc%d" % n, acc[:, n, :])
            for k in (0, 7):
                dump("u2T%d" % k, u2T[:, k, :])
            dump("gatesT", gatesT[0:32, :])
            return finish()
        P.barrier()
        hT = [cv(R3, 4096 + i * 1024, 1024, BF16, "p (e c t) -> p e c t", e=2, c=2) for i in range(2)]
        s_t = [cv(R3, 6144 + i * 512, 512) for i in range(2)]
        t_t = [cv(R3, 7168 + i * 256, 256, BF16) for i in range(2)]
        gsb = [cv(R3, 7680 + i * 256, 256, BF16) for i in range(2)]
        gate2_b = cv(R3, 8192, 1024)
        gB = [cv(R3, 9216 + i * 256, 256, BF16) for i in range(2 * EG)]
        build_gate_b(40, gate2_b)
        ln2w_b = cv(R3, 11264, 1024)
        ln2b_b = cv(R3, 12288, 1024)
        otb = [cv(R3, 13312, 1024), gate2_b]
        otk = ["ot0", "gate_b"]
        P.dma("sp", lambda e: e.dma_start(out=ln2w_b, in_=ln2w.partition_broadcast(128)), writes=["ln2w", "dg0", "dg1"])
        P.dma("sp", lambda e: e.dma_start(out=ln2b_b, in_=ln2b.partition_broadcast(128)), writes=["ln2b"])

        fin_st = {}

        def fin_a(tile):
            at = acc[:, tile, :]
            o = otb[tile % 2]
            ko = otk[tile % 2]
            fin_st[tile] = act_stats(at, "acc%d" % tile, 4 + tile % 2, o, ko)

        def fin_b(tile):
            at = acc[:, tile, :]
            ka = "acc%d" % tile
            o = otb[tile % 2]
            ko = otk[tile % 2]
            rstd, nb, sk = fin_st[tile]
            P.op("act", lambda e: e.activation(out=o, in_=at, func=AF.Identity, scale=rstd, bias=nb), reads=[ka] + sk, writes=[ko])
            P.op("dve", lambda e: e.tensor_tensor(out=o, in0=o, in1=ln2w_b, op=ALU.mult), reads=[ko, "ln2w"], writes=[ko])
            P.op("dve", lambda e: e.tensor_tensor(out=o, in0=o, in1=ln2b_b, op=ALU.add), reads=[ko, "ln2b"], writes=[ko])
            P.dma("sp", lambda e: e.dma_start(out=out[tile * 128:(tile + 1) * 128, :], in_=o), reads=[ko])

        def finalize_tile(tile):
            if tile >= 1:
                fin_b(tile - 1)
            fin_a(tile)


        def issue_gates(u):
            G_, tb_ = u // 4, u % 4
            for ei_ in range(EG):
                ex_ = G_ * EG + ei_
                sl_ = ei_ * 2 + u % 2
                P.dma("sp", lambda e, ex_=ex_, sl_=sl_, tb_=tb_: e.dma_start(out=gB[sl_], in_=gscr[ex_:ex_ + 1, tb_ * 512:(tb_ + 1) * 512].partition_broadcast(128)),
                      writes=["gB%d" % sl_])

        def scale_w2(slot):
            P.op("pool", lambda e: e.tensor_tensor(out=w2s[slot], in0=w2s[slot], in1=gate2_b.unsqueeze(1).to_broadcast([128, 2, 1024]), op=ALU.mult),
                 reads=["w2_%d" % slot, "gate_b"], writes=["w2_%d" % slot])

        NG = NE // EG
        for i in range(EG):
            load_expert(EG + i, EG + i)
        ucount = 0
        hcount = 0
        ycount = 0

        def stage2(G, tb, par, per_tile=None, inter=None):
            nonlocal ycount
            for ti in range(4):
                tile = tb * 4 + ti
                if per_tile is not None and ti >= 1:
                    per_tile(tile - 1)
                if inter is not None and ti >= 1 and inter:
                    inter.pop(0)()
                for hh in range(2):
                    by = 6 + ycount % 2
                    ycount += 1
                    idx = 0
                    for ei in range(EG):
                        slot = (G % 2) * EG + ei
                        for fc in range(2):
                            P.op("pe", lambda e, par=par, ei=ei, fc=fc, ti=ti, slot=slot, hh=hh, by=by, idx=idx: e.matmul(
                                pb(by), lhsT=hT[par][:, ei, fc, ti * 128:(ti + 1) * 128], rhs=w2s[slot][:, fc, hh * 512:(hh + 1) * 512],
                                start=(idx == 0), stop=(idx == 2 * EG - 1)),
                                reads=["hT%d" % par, "w2_%d" % slot], writes=["pb%d" % by], signal=(idx == 2 * EG - 1))
                            idx += 1
                    P.op("dve", lambda e, tile=tile, hh=hh, by=by: e.tensor_tensor(out=acc[:, tile, hh * 512:(hh + 1) * 512],
                                                                                 in0=acc[:, tile, hh * 512:(hh + 1) * 512], in1=pb(by), op=ALU.add),
                         reads=["pb%d" % by, "acc%d" % tile], writes=["acc%d" % tile])

        pending = None
        issue_gates(0)
        for G in range(NG):
            for tb in range(4):
                tbs = slice(tb * 512, (tb + 1) * 512)
                par = ucount % 2
                if ucount + 1 < NG * 4:
                    issue_gates(ucount + 1)
                ucount += 1
                if tb == 0:
                    for i in range(EG):
                        scale_w2((G % 2) * EG + i)
                s1 = []
                for ei in range(EG):
                  for fc in range(2):
                    s1.append((ei, fc))

                def sub_unit(ei, fc, G=G, tbs=tbs, par=par, uc=ucount):
                    nonlocal hcount
                    ex = G * EG + ei
                    slot = (G % 2) * EG + ei
                    bgt = 4 + (uc * EG + ei) % 2
                    gi = (uc * EG + ei) % 2
                    if True:
                        fs = slice(fc * 128, (fc + 1) * 128)
                        hb = hcount % 2
                        hcount += 1
                        for a in range(2):
                            bh = a * 2 + hb
                            for kc in range(8):
                                P.op("pe", lambda e, a=a, kc=kc, fs=fs, slot=slot, tbs=tbs, bh=bh: e.matmul(pb(bh), lhsT=w13[slot][:, a, kc, fs], rhs=u2T[:, kc, tbs],
                                                                                                    start=(kc == 0), stop=(kc == 7)),
                                     reads=["w13_%d" % slot, "u2T"], writes=["pb%d" % bh], signal=(kc == 7))
                        P.op("act", lambda e, hb=hb: e.activation(out=s_t[hb], in_=pb(hb), func=AF.Silu), reads=["pb%d" % hb], writes=["s_t%d" % hb])
                        P.op("dve", lambda e, hb=hb: e.tensor_tensor(out=t_t[hb], in0=pb(2 + hb), in1=s_t[hb], op=ALU.mult),
                             reads=["pb%d" % (2 + hb), "s_t%d" % hb], writes=["t_t%d" % hb])
                        P.op("pool", lambda e, hb=hb, par=par, ei=ei, fc=fc: e.tensor_tensor(out=hT[par][:, ei, fc, :], in0=t_t[hb], in1=gB[ei * 2 + par], op=ALU.mult),
                             reads=["t_t%d" % hb, "gB%d" % (ei * 2 + par)], writes=["hT%d" % par])
                thunks = [(lambda ei=ei, fc=fc: sub_unit(ei, fc)) for ei, fc in s1]
                last_grp = pending is not None and pending[0] == NG - 1 and debug != "B"
                if not last_grp:
                    for th in thunks:
                        th()
                    thunks = []
                else:
                    thunks.pop(0)()
                if pending is not None:
                    pG, ptb, _ = pending
                    fin = finalize_tile if (pG == NG - 1 and debug != "B") else None
                    stage2(*pending, per_tile=fin, inter=thunks)
                    while thunks:
                        thunks.pop(0)()
                    if fin is not None:
                        finalize_tile(ptb * 4 + 3)
                    if ptb == 3 and pG + 2 < NG:
                        for i in range(EG):
                            load_expert((pG + 2) * EG + i, (pG % 2) * EG + i)
                pump(2)
                pending = (G, tb, par)
        stage2(*pending, per_tile=(finalize_tile if debug != "B" else None))
        if debug != "B":
            finalize_tile(pending[1] * 4 + 3)
            fin_b(NT - 1)

        if debug == "B":
            for n in (0, 7, 15):
                dump("acc%d" % n, acc[:, n, :])
            return finish()
        P.barrier()
        P.wait_all("sp")
        P.emit(nc, st)
    return nc, consts


_CACHE = {}


def kernel(x, c, positions, w_ada, b_ada, w_in, ret_gn_w, w_pool, pool_scale, w_out,
           ln1_w, ln1_b, w_group, b_group, w_router, b_router, w1, w3, w2, ln2_w, ln2_b):
    if "nc" not in _CACHE:
        _CACHE["nc"] = build_nc()
    nc, consts = _CACHE["nc"]
    f = lambda a: np.ascontiguousarray(np.asarray(a, dtype=np.float32))
    x = f(x); c = f(c)
    positions = np.ascontiguousarray(np.asarray(positions, dtype=np.int32))
    shared = dict(
        w_ada=f(w_ada[0]), b_ada=f(b_ada[0])[None, :], w_in=f(w_in[0]), gnw=f(ret_gn_w[0])[None, :],
        w_pool=f(w_pool[0]), pscale=f(np.asarray(pool_scale[0]).reshape(4, 128).T), w_out=f(w_out[0]),
        ln1w=f(ln1_w[0])[None, :], ln1b=f(ln1_b[0])[None, :], ln2w=f(ln2_w[0])[None, :], ln2b=f(ln2_b[0])[None, :],
        wr=f(np.concatenate([np.asarray(w_router[0]), np.asarray(w_group[0])], axis=1)),
        br=f(np.concatenate([np.asarray(b_router[0]), np.asarray(b_group[0])]))[None, :],
        w1=f(np.asarray(w1[0]).reshape(NE, D, 256)), w3=f(np.asarray(w3[0]).reshape(NE, D, 256)),
        w2=f(np.asarray(w2[0]).reshape(NE, 256, D)),
        **consts,
    )
    in_maps = []
    for b in range(8):
        m = dict(shared)
        m["x"] = x[b]
        m["c_col"] = f(c[b].reshape(8, 128).T)
        m["pos"] = positions[b][None, :]
        in_maps.append(m)
    res = run_bass_kernel_spmd(nc, in_maps, core_ids=list(range(8)))
    return np.stack([np.asarray(r["out"], dtype=np.float32) for r in res.results], axis=0)
```

```python
import numpy as np
from contextlib import ExitStack
import concourse.bass as bass
import concourse.mybir as mybir
from concourse.bass_utils import run_bass_kernel_spmd

F32 = mybir.dt.float32
BF16 = mybir.dt.bfloat16
I32 = mybir.dt.int32
ALU = mybir.AluOpType
AF = mybir.ActivationFunctionType
AX = mybir.AxisListType

T = 2048
D = 1024
NT = 16
KD = 8
H = 4
NE = 32
ALPHA = 2.0 ** 0.25
LN_EPS = 1e-5
POOL_WINDOWS = (2, 4, 8, 16)
BIG = 1.0e9
TWO_PI = float(2.0 * np.pi)
EG = 2

ENGS = ("pe", "act", "dve", "pool", "sp")


class Prog:
    def __init__(self, n_dma_sems=16):
        self.ops = {e: [] for e in ENGS}
        self.cnt = {e: 0 for e in ENGS}
        self.seen = {e: {} for e in ENGS}
        self.pend = {e: {} for e in ENGS}
        self.res = {}
        self.n_dma = n_dma_sems
        self.dma_val = {}
        self.dma_rr = {e: 0 for e in ENGS}

    def _need(self, eng, waits, dep):
        if dep is None:
            return
        key, val = dep
        if key == eng and eng == "pe":
            return
        if self.seen[eng].get(key, 0) >= val:
            return
        if waits.get(key, 0) < val:
            waits[key] = val

    def _track(self, eng, waits, reads, writes, dep):
        for r in reads:
            ent = self.res.setdefault(r, {"w": None, "r": {}})
            self._need(eng, waits, ent["w"])
        for w in writes:
            ent = self.res.setdefault(w, {"w": None, "r": {}})
            self._need(eng, waits, ent["w"])
            for k, v in ent["r"].items():
                self._need(eng, waits, (k, v))
        for r in reads:
            ent = self.res[r]
            if ent["r"].get(dep[0], 0) < dep[1]:
                ent["r"][dep[0]] = dep[1]
        for w in writes:
            self.res[w] = {"w": dep, "r": {}}

    def _finish_waits(self, eng, waits):
        for k, v in self.pend[eng].items():
            if self.seen[eng].get(k, 0) < v and waits.get(k, 0) < v:
                waits[k] = v
        self.pend[eng] = {}
        for k, v in waits.items():
            self.seen[eng][k] = v
        return list(waits.items())

    def op(self, eng, fn, reads=(), writes=(), signal=True):
        if eng in ("act", "dve"):
            pr = [r for r in reads if r.startswith("pb")]
            if pr:
                reads = [r for r in reads if not r.startswith("pb")]
                writes = list(writes) + pr
        waits = {}
        dep = (eng, self.cnt[eng] + 1)
        self._track(eng, waits, reads, writes, dep)
        wl = self._finish_waits(eng, waits)
        if signal:
            self.cnt[eng] += 1
        self.ops[eng].append((wl, fn, "inc" if signal else None))

    def dma(self, eng, fn, reads=(), writes=()):
        i = self.dma_rr[eng]
        self.dma_rr[eng] = (i + 1) % self.n_dma
        key = ("d", eng, i)
        prev = self.dma_val.get(key, 0)
        waits = {}
        if prev:
            self._need(eng, waits, (key, prev))
        val = prev + 16
        self.dma_val[key] = val
        self._track(eng, waits, reads, writes, (key, val))
        wl = self._finish_waits(eng, waits)
        self.ops[eng].append((wl, fn, key))

    def barrier(self):
        cur = dict(self.cnt)
        dm = dict(self.dma_val)
        for e in ENGS:
            for k, v in cur.items():
                if k != e and v > 0 and self.pend[e].get(k, 0) < v:
                    self.pend[e][k] = v
            for k, v in dm.items():
                if self.pend[e].get(k, 0) < v:
                    self.pend[e][k] = v
        self.res = {}

    def wait_all(self, eng):
        for k, v in self.cnt.items():
            if k != eng and v > 0:
                self.pend[eng][k] = max(self.pend[eng].get(k, 0), v)
        for k, v in self.dma_val.items():
            self.pend[eng][k] = max(self.pend[eng].get(k, 0), v)

    def emit(self, nc, stack):
        sems = {}
        for e in ENGS:
            sems[e] = stack.enter_context(nc.semaphore("s_" + e))
        for key in self.dma_val:
            sems[key] = stack.enter_context(nc.semaphore("d_%s_%d" % (key[1], key[2])))
        block = stack.enter_context(nc.Block())
        ops = self.ops
        pend = self.pend

        def run(engname):
            def body(engine):
                for wl, fn, sig in ops[engname]:
                    for k, v in wl:
                        engine.wait_ge(sems[k], v)
                    ins = fn(engine)
                    if sig == "inc":
                        ins.then_inc(sems[engname], 1)
                    elif sig is not None:
                        ins.then_inc(sems[sig], 16)
                for k, v in pend[engname].items():
                    engine.wait_ge(sems[k], v)
            return body

        block.tensor(run("pe"))
        block.scalar(run("act"))
        block.vector(run("dve"))
        block.gpsimd(run("pool"))
        block.sync(run("sp"))


def _consts():
    lg = np.log1p(-np.exp2(-5.0 - np.arange(H, dtype=np.float64)))
    m = np.arange(128, dtype=np.float64)
    scale = 128.0 ** -0.5
    mask = np.zeros((128, H, 128), np.float64)
    for h in range(H):
        mask[:, h, :] = np.where(m[None, :] >= m[:, None], np.exp(-(m[:, None] + 1.0) * lg[h]) * scale, 0.0)
    qdec = np.zeros((128, H, 128), np.float64)
    for h in range(H):
        qdec[:, h, :] = np.exp((m[None, :] + 1.0) * lg[h])
    vdec = np.exp((127.0 - m[:, None]) * lg[None, :]) * scale
    cd = [float(np.exp(128.0 * lg[h])) for h in range(H)]
    inv_freq = 10000.0 ** (-np.arange(0, 128, 2, dtype=np.float64) / 128.0)
    invf = (np.concatenate([inv_freq, inv_freq]) / (2 * np.pi))[:, None]
    perm = np.zeros((128, 128), np.float64)
    for j in range(64):
        perm[j + 64, j] = -1.0
        perm[j, j + 64] = 1.0
    amat = np.zeros((128, 3, 4, 128), np.float64)
    for g, w in enumerate(POOL_WINDOWS):
        for t in range(128):
            cnt0 = min(t + 1, w)
            for s in range(max(0, t - w + 1), t + 1):
                amat[s, 0, g, t] += 1.0 / cnt0
                amat[s, 1, g, t] += 1.0 / w
            amat[t, 0, g, t] -= 1.0
            amat[t, 1, g, t] -= 1.0
            for j in range(t + 1, w):
                amat[128 + t - j, 2, g, t] += 1.0 / w
    f = lambda a: np.ascontiguousarray(a.astype(np.float32))
    return dict(ident=f(np.eye(128)), perm=f(perm), mask=f(mask.reshape(128, 512)), qdec=f(qdec.reshape(128, 512)),
                vdec=f(vdec), invf=f(invf), amat=f(amat.reshape(128, 1536))), cd


def build_nc(debug=None):
    consts, CD = _consts()
    dumps = []
    nc = bass.Bass("TRN2", target_bir_lowering=False)

    def din(name, shape, dt=F32):
        return nc.dram_tensor(name, list(shape), dt, kind="ExternalInput").ap()

    x = din("x", [T, D])
    c_col = din("c_col", [128, 8])
    pos = din("pos", [1, T], I32)
    w_ada = din("w_ada", [D, 6 * D])
    b_ada = din("b_ada", [1, 6 * D])
    w_in = din("w_in", [D, 2560])
    gnw = din("gnw", [1, 512])
    w_pool = din("w_pool", [4, 128, 128])
    pscale = din("pscale", [128, 4])
    w_out = din("w_out", [D, D])
    ln1w = din("ln1w", [1, D]); ln1b = din("ln1b", [1, D])
    ln2w = din("ln2w", [1, D]); ln2b = din("ln2b", [1, D])
    wr = din("wr", [D, 36]); br = din("br", [1, 36])
    w1 = din("w1", [NE, D, 256]); w3 = din("w3", [NE, D, 256]); w2 = din("w2", [NE, 256, D])
    c_ident = din("ident", [128, 128]); c_perm = din("perm", [128, 128])
    c_mask = din("mask", [128, 512]); c_qdec = din("qdec", [128, 512]); c_vdec = din("vdec", [128, 4])
    c_invf = din("invf", [128, 1]); c_amat = din("amat", [128, 1536])
    out = nc.dram_tensor("out", [T, D], F32, kind="ExternalOutput").ap()
    gscr = nc.dram_tensor("gscr", [32, T], BF16, kind="Internal").ap()

    P = Prog()
    st = ExitStack()
    with st:
        R0 = st.enter_context(nc.sbuf_tensor("R0", [128, 16384], F32))
        R1 = st.enter_context(nc.sbuf_tensor("R1", [128, 8192], F32))
        R2 = st.enter_context(nc.sbuf_tensor("R2", [128, 8192], F32))
        R3 = st.enter_context(nc.sbuf_tensor("R3", [128, 15360], F32))
        RC = st.enter_context(nc.sbuf_tensor("RC", [128, 1792], F32))
        G1 = st.enter_context(nc.sbuf_tensor("G1", [128, 1280], F32))
        pbs = [st.enter_context(nc.psum_tensor("pb%d" % i, [128, 512], F32)) for i in range(8)]

        def pb(i):
            return pbs[i][:, :]

        def pbb(i):
            return pbs[i][:, :].bitcast(BF16)

        def cv(R, off, ncol, dt=F32, pat=None, **kw):
            v = R[:, off:off + ncol]
            if dt != F32:
                v = v.bitcast(dt)
            if pat:
                v = v.rearrange(pat, **kw)
            return v

        def dump(name, ap):
            shp = list(ap.shape)
            d = nc.dram_tensor("dbg_" + name, shp, F32, kind="ExternalOutput").ap()
            P.barrier()
            P.dma("pool", lambda e: e.dma_start(out=d, in_=ap))
            dumps.append("dbg_" + name)

        def finish():
            P.barrier()
            P.wait_all("sp")
            P.emit(nc, st)
            return nc, consts

        ident = cv(RC, 0, 128)
        identb = cv(RC, 128, 64, BF16)
        permb = cv(RC, 192, 64, BF16)
        ones_f = cv(RC, 256, 128)
        adaCol = cv(RC, 384, 48)
        ccol = cv(RC, 432, 8)
        cbf = cv(RC, 440, 4, BF16)
        vdec = cv(RC, 444, 4)
        invf = cv(RC, 448, 1)
        negh = cv(RC, 449, 4)
        psc = cv(RC, 453, 4)
        one11 = cv(RC, 457, 1)
        wr_sb = cv(RC, 460, 288, F32, "p (k n) -> p k n", k=8)
        br_b = cv(RC, 748, 36)
        stat = cv(RC, 784, 64)
        rt_ = cv(RC, 848, 160)
        permf = cv(RC, 1008, 128)
        stat2 = cv(RC, 1136, 192)
        rt_alt = cv(RC, 1328, 160)
        gst_all = cv(RC, 1488, 256)
        gatesT = cv(R3, 14336, 1024, BF16)

        P.dma("sp", lambda e: e.dma_start(out=ident, in_=c_ident), writes=["ident"])
        P.dma("sp", lambda e: e.dma_start(out=permf, in_=c_perm), writes=["permf"])
        P.dma("sp", lambda e: e.dma_start(out=ccol, in_=c_col), writes=["ccol"])
        P.dma("sp", lambda e: e.dma_start(out=vdec, in_=c_vdec), writes=["vdec"])
        P.dma("sp", lambda e: e.dma_start(out=invf, in_=c_invf), writes=["invf"])
        P.dma("sp", lambda e: e.dma_start(out=psc, in_=pscale), writes=["psc"])
        P.dma("sp", lambda e: e.dma_start(out=wr_sb, in_=wr.rearrange("(k p) n -> p k n", p=128)), writes=["wr"])
        P.dma("sp", lambda e: e.dma_start(out=br_b, in_=br.partition_broadcast(128)), writes=["brb"])
        P.op("dve", lambda e: e.tensor_copy(out=identb, in_=ident), reads=["ident"], writes=["identb"])
        P.op("dve", lambda e: e.tensor_copy(out=permb, in_=permf), reads=["permf"], writes=["permb"])
        P.op("pool", lambda e: e.memset(ones_f, 1.0), writes=["ones"])
        P.op("pool", lambda e: e.memset(negh, -0.5), writes=["negh"])
        P.op("pool", lambda e: e.memset(one11, 1.0), writes=["one11"])
        P.op("act", lambda e: e.activation(out=cbf, in_=ccol, func=AF.Silu), reads=["ccol"], writes=["cbf"])

        def ln_stats(src, key, slot, want_nmr=False):
            base = slot * 24
            sb = stat2[:, base: base + 12]
            mv = stat2[:, base + 12: base + 14]
            ve = stat2[:, base + 14: base + 15]
            rs = stat2[:, base + 15: base + 16]
            nm = stat2[:, base + 16: base + 17]
            k = "stat%d" % slot
            for hh in range(2):
                P.op("dve", lambda e, hh=hh: e.bn_stats(out=sb[:, hh * 6:(hh + 1) * 6], in_=src[:, hh * 512:(hh + 1) * 512]),
                     reads=[key], writes=[k + "a"])
            P.op("dve", lambda e: e.bn_aggr(out=mv, in_=sb), reads=[k + "a"], writes=[k + "mv"])
            P.op("pool", lambda e: e.tensor_scalar(out=ve, in0=mv[:, 1:2], scalar1=LN_EPS, scalar2=1.0, op0=ALU.add, op1=ALU.mult), reads=[k + "mv"], writes=[k + "ve"])
            P.op("pool", lambda e: e.tensor_tensor(out=rs, in0=ve, in1=negh[:, 0:1], op=ALU.pow), reads=[k + "ve", "negh"], writes=[k + "rs"])
            if want_nmr:
                P.op("dve", lambda e: e.scalar_tensor_tensor(out=nm, in0=mv[:, 0:1], scalar=-1.0, in1=rs, op0=ALU.mult, op1=ALU.mult),
                     reads=[k + "mv", k + "rs"], writes=[k + "nm"])
                return mv[:, 0:1], rs, nm, [k + "mv", k + "rs", k + "nm"]
            return mv[:, 0:1], rs, [k + "mv", k + "rs"]

        def ln_stats_sqrt(src, key, slot):
            base = slot * 24
            sb = stat2[:, base: base + 12]
            mv = stat2[:, base + 12: base + 14]
            sd = stat2[:, base + 14: base + 15]
            rs = stat2[:, base + 15: base + 16]
            k = "stat%d" % slot
            for hh in range(2):
                P.op("dve", lambda e, hh=hh: e.bn_stats(out=sb[:, hh * 6:(hh + 1) * 6], in_=src[:, hh * 512:(hh + 1) * 512]),
                     reads=[key], writes=[k + "a"])
            P.op("dve", lambda e: e.bn_aggr(out=mv, in_=sb), reads=[k + "a"], writes=[k + "mv"])
            P.op("act", lambda e: e.activation(out=sd, in_=mv[:, 1:2], func=AF.Sqrt, bias=LN_EPS, scale=1.0), reads=[k + "mv"], writes=[k + "ve"])
            return mv[:, 0:1], sd, rs, k

        def act_stats(src, ksrc, slot, junk, kjunk, alpha=None):
            base = slot * 24
            s2_ = stat2[:, base + 0: base + 1]
            s1_ = stat2[:, base + 1: base + 2]
            ngm = stat2[:, base + 2: base + 3]
            m2_ = stat2[:, base + 3: base + 4]
            ve_ = stat2[:, base + 4: base + 5]
            rs_ = stat2[:, base + 15: base + 16]
            nb_ = stat2[:, base + 16: base + 17]
            sc_ = stat2[:, base + 17: base + 18]
            k = "as%d" % slot
            a_ = 1.0 if alpha is None else alpha
            P.op("act", lambda e: e.activation(out=junk, in_=src, func=AF.Square, accum_out=s2_), reads=[ksrc], writes=[kjunk, k + "s2"])
            if alpha is None:
                P.op("act", lambda e: e.activation(out=junk, in_=src, func=AF.Identity, accum_out=s1_), reads=[ksrc], writes=[kjunk, k + "s1"])
            else:
                P.op("act", lambda e: e.activation(out=src, in_=src, func=AF.Copy, scale=alpha, accum_out=s1_), reads=[ksrc], writes=[ksrc, k + "s1"])
            P.op("pool", lambda e: e.tensor_scalar(out=ngm, in0=s1_, scalar1=-1.0 / (1024.0 * a_), scalar2=1.0, op0=ALU.mult, op1=ALU.mult),
                 reads=[k + "s1"], writes=[k + "ngm"])
            P.op("pool", lambda e: e.tensor_tensor(out=m2_, in0=ngm, in1=ngm, op=ALU.mult), reads=[k + "ngm"], writes=[k + "m2"])
            P.op("pool", lambda e: e.tensor_scalar(out=ve_, in0=s2_, scalar1=1.0 / 1024.0, scalar2=LN_EPS, op0=ALU.mult, op1=ALU.add),
                 reads=[k + "s2"], writes=[k + "ve"])
            P.op("pool", lambda e: e.tensor_tensor(out=ve_, in0=ve_, in1=m2_, op=ALU.subtract), reads=[k + "ve", k + "m2"], writes=[k + "ve"])
            P.op("pool", lambda e: e.tensor_tensor(out=rs_, in0=ve_, in1=negh[:, 0:1], op=ALU.pow), reads=[k + "ve", "negh"], writes=[k + "rs"])
            P.op("pool", lambda e: e.tensor_tensor(out=nb_, in0=ngm, in1=rs_, op=ALU.mult), reads=[k + "ngm", k + "rs"], writes=[k + "nb"])
            if alpha is None:
                return rs_, nb_, [k + "rs", k + "nb"]
            P.op("pool", lambda e: e.tensor_scalar(out=sc_, in0=rs_, scalar1=1.0 / alpha, scalar2=1.0, op0=ALU.mult, op1=ALU.mult),
                 reads=[k + "rs"], writes=[k + "sc"])
            return sc_, nb_, [k + "sc", k + "nb"]

        uT = cv(R0, 0, 8192, BF16, "p (k t) -> p k t", k=8)
        qT = cv(R0, 8192, 4096, BF16, "p (h t) -> p h t", h=4)
        krT = cv(R0, 12288, 4096, BF16, "p (h t) -> p h t", h=4)
        k_tok = cv(R1, 0, 4096, BF16, "p (n f) -> p n f", n=16)
        v_tok = cv(R1, 4096, 4096, BF16, "p (n f) -> p n f", n=16)
        catT = cv(R2, 0, 8192, BF16, "p (k t) -> p k t", k=8)
        prev = cv(R3, 0, 4096, BF16, "p (n f) -> p n f", n=16)
        adaslot = [cv(R2, i * 2048, 2048, BF16, "p (k n) -> p k n", k=8) for i in range(4)]
        wg = cv(R3, 4096, 2048, BF16, "p (k n) -> p k n", k=8)
        TB = 6144

        arow = [cv(R1, i * 512, 512) for i in range(2)]
        brow = [cv(R1, 1024, 512), cv(R1, 1536, 512), cv(R3, 0, 512), cv(R3, 512, 512)]

        def asl(j):
            return j if j < 4 else j % 2

        def ada_load(j):
            s = asl(j)
            P.dma("pool", lambda e: e.dma_start(out=adaslot[s], in_=w_ada[:, j * 512:(j + 1) * 512].rearrange("(k p) n -> p k n", p=128)),
                  writes=["adaslot%d" % s])
            P.dma("sp", lambda e: e.dma_start(out=brow[s][0:1, :], in_=b_ada[:, j * 512:(j + 1) * 512]), writes=["brow%d" % s])

        def ada_chunk(j):
            ada_compute(j)
            if j >= 4 and j + 2 < 12:
                ada_load(j + 2)

        def ada_compute(j):
            s = j % 2
            sl_ = asl(j)
            for k in range(8):
                P.op("pe", lambda e, k=k: e.matmul(pb(7)[0:1, :], lhsT=cbf[:, k:k + 1], rhs=adaslot[sl_][:, k, :], start=(k == 0), stop=(k == 7)),
                     reads=["cbf", "adaslot%d" % sl_], writes=["pb7"], signal=(k == 7))
            P.op("dve", lambda e: e.tensor_tensor(out=arow[s][0:1, :], in0=pb(7)[0:1, :], in1=brow[sl_][0:1, :], op=ALU.add),
                 reads=["pb7", "brow%d" % sl_], writes=["arow%d" % s])
            for i in range(4):
                col = j * 4 + i
                P.op("pe", lambda e, i=i, col=col: e.matmul(pb(6)[:, col:col + 1], lhsT=arow[s][0:1, i * 128:(i + 1) * 128], rhs=one11[0:1, 0:1],
                                                          start=True, stop=True),
                     reads=["arow%d" % s, "one11"], writes=["pb6"], signal=(i == 3))
            c0, c1 = j * 4, j * 4 + 4
            addv = 1.0 if j in (2, 3, 8, 9) else 0.0
            P.op("dve", lambda e: e.tensor_scalar_add(out=adaCol[:, c0:c1], in0=pb(6)[:, c0:c1], scalar1=addv), reads=["pb6"], writes=["adaCol"])

        wslot = [cv(R3, TB + i * 2048, 2048, BF16, "p (k n) -> p k n", k=8) for i in range(2)]

        def load_win(blk, dst, key):
            P.dma("pool", lambda e: e.dma_start(out=dst, in_=w_in[:, blk * 512:(blk + 1) * 512].rearrange("(k p) n -> p k n", p=128)),
                  writes=[key])

        for j in range(4):
            ada_load(j)
        wslotA = [cv(R0, 8192 + i * 2048, 2048, BF16, "p (k n) -> p k n", k=8) for i in range(2)]

        xin = [cv(R3, TB + i * 1024, 1024) for i in range(3)]
        xn = [cv(R3, TB + 3072 + i * 512, 512, BF16) for i in range(4)]
        a1s = {}

        def a1_stats(tile):
            xb = xin[tile % 3]
            kx = "xin%d" % (tile % 3)
            P.dma("sp", lambda e: e.dma_start(out=xb, in_=x[tile * 128:(tile + 1) * 128, :]), writes=[kx])
            a1s[tile] = ln_stats_sqrt(xb, kx, tile % 2)

        def a1_norm(tile):
            i = tile % 4
            blk = tile // 4
            bo = (blk % 2) * 4
            xb = xin[tile % 3]
            kx = "xin%d" % (tile % 3)
            mean, sd_, rstd, k_ = a1s[tile]
            P.op("dve", lambda e: e.reciprocal(out=rstd, in_=sd_), reads=[k_ + "ve"], writes=[k_ + "rs"])
            P.op("dve", lambda e: e.tensor_scalar(out=xn[i], in0=xb, scalar1=mean, scalar2=rstd, op0=ALU.subtract, op1=ALU.mult),
                 reads=[kx, k_ + "mv", k_ + "rs"], writes=["xn%d" % i])
            for kc in range(8):
                P.op("pe", lambda e, kc=kc: e.transpose(out=pbb(bo + kc // 2)[:, (kc % 2) * 512 + i * 128:(kc % 2) * 512 + (i + 1) * 128],
                                                        in_=xn[i][:, kc * 128:(kc + 1) * 128], identity=identb),
                     reads=["xn%d" % i, "identb"], writes=["pb%d" % (bo + kc // 2)], signal=(kc == 7))
            if i == 3:
                if blk == 0:
                    for j in range(4):
                        ada_chunk(j)
                for kc in range(8):
                    P.op("act", lambda e, kc=kc: e.activation(out=uT[:, kc, blk * 512:(blk + 1) * 512],
                                                            in_=pbb(bo + kc // 2)[:, (kc % 2) * 512:(kc % 2 + 1) * 512],
                                                            func=AF.Identity, scale=adaCol[:, 8 + kc:9 + kc], bias=adaCol[:, kc:kc + 1]),
                         reads=["pb%d" % (bo + kc // 2), "adaCol"], writes=["uT"])
                if blk == 2:
                    load_win(2, wslotA[0], "wsA0")
                    load_win(4, wslotA[1], "wsA1")

        for t_ in range(NT + 1):
            if t_ < NT:
                a1_stats(t_)
            if t_ >= 1:
                a1_norm(t_ - 1)

        if debug == "A1":
            dump("adaCol", adaCol)
            for k in range(8):
                dump("uT%d" % k, uT[:, k, :])
            return finish()
        P.barrier()
        load_win(0, wslot[0], "ws0")
        load_win(1, wslot[1], "ws1")
        ada_load(4)
        ada_load(5)
        load_win(3, wg, "wg")
        cs_all = cv(R3, 0, 2048)
        sn_all = cv(R3, 2048, 2048)
        t_posi = cv(R1, 2048, 512, I32)
        t_posf = cv(R1, 2560, 512)
        t_rr = cv(R1, 3072, 512)
        t_nf = cv(R1, 3584, 512)
        t_ni = t_posi
        tab_thunks = []
        for tb_ in range(4):
            tbs_ = slice(tb_ * 512, (tb_ + 1) * 512)
            tab_thunks.append(lambda tbs_=tbs_: P.dma("sp", lambda e: e.dma_start(out=t_posi, in_=pos[:, tbs_].partition_broadcast(128)), writes=["posi"]))
            tab_thunks.append(lambda: P.op("dve", lambda e: e.tensor_copy(out=t_posf, in_=t_posi), reads=["posi"], writes=["posf"]))
            for dst_, key_, off_ in ((sn_all, "sn_all", 0.0), (cs_all, "cs_all", 0.25)):
                d_ = dst_[:, tbs_]
                tab_thunks.append(lambda off_=off_: P.op("dve", lambda e: e.tensor_scalar(out=t_rr, in0=t_posf, scalar1=invf[:, 0:1], scalar2=off_, op0=ALU.mult, op1=ALU.add),
                                                         reads=["posf", "invf"], writes=["rr"]))
                tab_thunks.append(lambda: P.op("dve", lambda e: e.tensor_copy(out=t_ni, in_=t_rr), reads=["rr", "posf"], writes=["posi"]))
                tab_thunks.append(lambda: P.op("dve", lambda e: e.tensor_copy(out=t_nf, in_=t_ni), reads=["posi"], writes=["nf"]))
                tab_thunks.append(lambda: P.op("dve", lambda e: e.tensor_tensor(out=t_rr, in0=t_rr, in1=t_nf, op=ALU.subtract), reads=["rr", "nf"], writes=["rr"]))
                tab_thunks.append(lambda: P.op("dve", lambda e: e.tensor_single_scalar(out=t_nf, in_=t_rr, scalar=0.5, op=ALU.is_gt), reads=["rr"], writes=["nf"]))
                tab_thunks.append(lambda: P.op("dve", lambda e: e.tensor_tensor(out=t_rr, in0=t_rr, in1=t_nf, op=ALU.subtract), reads=["rr", "nf"], writes=["rr"]))
                tab_thunks.append(lambda: P.op("dve", lambda e: e.tensor_single_scalar(out=t_nf, in_=t_rr, scalar=-0.5, op=ALU.is_lt), reads=["rr"], writes=["nf"]))
                tab_thunks.append(lambda d_=d_, key_=key_: P.op("dve", lambda e: e.tensor_tensor(out=d_, in0=t_rr, in1=t_nf, op=ALU.add), reads=["rr", "nf"], writes=[key_]))
        p_tok = [cv(R3, TB + 4096 + i * 512, 512) for i in range(2)]
        pooledT = cv(R3, TB + 5120, 1024, BF16, "p (g t) -> p g t", g=4)
        amat = cv(R3, TB + 6144, 1536, F32, "p (k g t) -> p k g t", k=3, g=4)
        wpool = cv(R3, TB + 7680, 256, BF16, "p (g d) -> p g d", g=4)
        P.dma("sp", lambda e: e.dma_start(out=amat, in_=c_amat.rearrange("p (k g t) -> p k g t", k=3, g=4)), writes=["amat"])
        P.dma("pool", lambda e: e.dma_start(out=wpool, in_=w_pool.rearrange("g c d -> c g d")), writes=["wpool"])
        for tile in range(NT):
            ts = slice(tile * 128, (tile + 1) * 128)
            bv = tile % 2
            for kc in range(8):
                P.op("pe", lambda e, kc=kc, ts=ts, bv=bv: e.matmul(pb(bv), lhsT=uT[:, kc, ts], rhs=wslotA[0][:, kc, :], start=(kc == 0), stop=(kc == 7)),
                     reads=["uT", "wsA0"], writes=["pb%d" % bv], signal=(kc == 7))
            P.op("act", lambda e, tile=tile, bv=bv: e.activation(out=v_tok[:, tile, :], in_=pb(bv), func=AF.Copy), reads=["pb%d" % bv], writes=["v_tok"])
            bp = 2 + tile % 2
            for kc in range(8):
                P.op("pe", lambda e, kc=kc, ts=ts, bp=bp: e.matmul(pb(bp), lhsT=uT[:, kc, ts], rhs=wslotA[1][:, kc, :], start=(kc == 0), stop=(kc == 7)),
                     reads=["uT", "wsA1"], writes=["pb%d" % bp], signal=(kc == 7))
            pc = p_tok[tile % 2]
            pp = p_tok[(tile - 1) % 2]
            P.op("act", lambda e, pc=pc, bp=bp: e.activation(out=pc, in_=pb(bp), func=AF.Copy), reads=["pb%d" % bp], writes=["ptok%d" % (tile % 2)])
            bq = 4 + tile % 2
            for g in range(4):
                gs = slice(g * 128, (g + 1) * 128)
                kind = 0 if tile == 0 else 1
                P.op("pe", lambda e, g=g, gs=gs, pc=pc, bq=bq, kind=kind, tile=tile: e.matmul(pb(bq)[:, gs], lhsT=pc[:, gs], rhs=amat[:, kind, g, :],
                                                                                           start=True, stop=(tile == 0)),
                     reads=["ptok%d" % (tile % 2), "amat"], writes=["pb%d" % bq], signal=(tile == 0 and g == 3))
                if tile > 0:
                    P.op("pe", lambda e, g=g, gs=gs, pp=pp, bq=bq: e.matmul(pb(bq)[:, gs], lhsT=pp[:, gs], rhs=amat[:, 2, g, :], start=False, stop=True),
                         reads=["ptok%d" % ((tile - 1) % 2), "amat"], writes=["pb%d" % bq], signal=(g == 3))
            for _ in range(5):
                if tab_thunks:
                    tab_thunks.pop(0)()
            ti = tile % 4
            P.op("act", lambda e, bq=bq, ti=ti: e.activation(out=pooledT[:, :, ti * 128:(ti + 1) * 128],
                                                            in_=pb(bq).rearrange("p (g t) -> p g t", g=4), func=AF.Copy),
                 reads=["pb%d" % bq], writes=["pooledT"])
            if ti == 3:
                ada_chunk(4 + tile // 4)
            if ti == 3:
                blk = tile // 4
                for g in range(4):
                    bw = 6 + g % 2
                    P.op("pe", lambda e, g=g, bw=bw: e.matmul(pb(bw), lhsT=wpool[:, g, :], rhs=pooledT[:, g, :], start=True, stop=True),
                         reads=["wpool", "pooledT"], writes=["pb%d" % bw])
                    P.op("act", lambda e, g=g, bw=bw, blk=blk: e.activation(out=catT[:, 4 + g, blk * 512:(blk + 1) * 512], in_=pb(bw),
                                                                          func=AF.Identity, scale=psc[:, g:g + 1]),
                         reads=["pb%d" % bw, "psc"], writes=["catT"])

        while tab_thunks:
            tab_thunks.pop(0)()
        P.op("act", lambda e: e.activation(out=sn_all, in_=sn_all, func=AF.Sin, scale=TWO_PI), reads=["sn_all"], writes=["sn_all"])
        P.op("act", lambda e: e.activation(out=cs_all, in_=cs_all, func=AF.Sin, scale=TWO_PI), reads=["cs_all"], writes=["cs_all"])
        if debug == "A2a":
            for n in range(0, 16, 5):
                dump("v%d" % n, v_tok[:, n, :])
            for k in range(4, 8):
                dump("catT%d" % k, catT[:, k, :])
            return finish()
        gate1_b = G1[:, 0:1024]
        dg1 = G1[:, 1024:1280]

        def build_gate_b_early(col0, gdst, dgb, bank0, key):
            for kc in range(8):
                d_ = dgb[:, (kc % 2) * 128:(kc % 2 + 1) * 128]
                P.op("dve", lambda e, kc=kc, d_=d_: e.tensor_scalar(out=d_, in0=ident, scalar1=adaCol[:, col0 + kc:col0 + kc + 1], scalar2=None, op0=ALU.mult),
                     reads=["ident", "adaCol"], writes=["dge%d" % (kc % 2)])
                P.op("pe", lambda e, kc=kc, d_=d_: e.matmul(pb(bank0 + kc // 4)[:, (kc % 4) * 128:(kc % 4 + 1) * 128], lhsT=ones_f, rhs=d_, start=True, stop=True),
                     reads=["ones", "dge%d" % (kc % 2)], writes=["pb%d" % (bank0 + kc // 4)])
            for hh in range(2):
                P.op("act", lambda e, hh=hh: e.activation(out=gdst[:, hh * 512:(hh + 1) * 512], in_=pb(bank0 + hh), func=AF.Copy),
                     reads=["pb%d" % (bank0 + hh)], writes=[key])

        P.barrier()
        qdec = cv(R3, 14080, 512, F32, "p (h c) -> p h c", h=4)
        qb = [cv(R3, TB + 4096 + i * 256, 256, BF16) for i in range(2)]
        rt1 = [cv(R3, TB + 4608 + i * 512, 512) for i in range(2)]
        rt2 = [cv(R3, TB + 5632 + i * 512, 512) for i in range(2)]
        P.dma("sp", lambda e: e.dma_start(out=qdec, in_=c_qdec.rearrange("p (h c) -> p h c", h=4)), writes=["qdec"])
        build_gate_b_early(16, gate1_b, dg1, 4, "gate1b")
        units = [(tb, which, h) for which in range(2) for tb in range(4) for h in range(4)]

        def u_s0(u):
            tb, which, h = units[u]
            tbs = slice(tb * 512, (tb + 1) * 512)
            hs = slice(h * 128, (h + 1) * 128)
            ba = u % 2
            cb = u % 2
            for kc in range(8):
                P.op("pe", lambda e, kc=kc: e.matmul(pb(ba), lhsT=wslot[which][:, kc, hs], rhs=uT[:, kc, tbs], start=(kc == 0), stop=(kc == 7)),
                     reads=["ws%d" % which, "uT"], writes=["pb%d" % ba], signal=(kc == 7))
            P.op("act", lambda e: e.activation(out=qb[cb], in_=pb(ba), func=AF.Copy), reads=["pb%d" % ba], writes=["qb%d" % cb])

        def u_s1(u):
            tb, which, h = units[u]
            tbs = slice(tb * 512, (tb + 1) * 512)
            tp_ = tb % 2
            ba = u % 2
            bb = 2 + u % 2
            cb = u % 2
            P.op("pe", lambda e: e.matmul(pb(bb), lhsT=permb, rhs=qb[cb], start=True, stop=True), reads=["permb", "qb%d" % cb], writes=["pb%d" % bb])
            P.op("dve", lambda e: e.tensor_tensor(out=rt1[cb], in0=pb(ba), in1=cs_all[:, tbs], op=ALU.mult),
                 reads=["pb%d" % ba, "qb%d" % cb], writes=["rt1%d" % cb])
            P.op("dve", lambda e: e.tensor_tensor(out=rt2[cb], in0=pb(bb), in1=sn_all[:, tbs], op=ALU.mult),
                 reads=["pb%d" % bb], writes=["rt2%d" % cb])
            if which == 0:
                P.op("pool", lambda e: e.tensor_tensor(out=rt1[cb], in0=rt1[cb], in1=rt2[cb], op=ALU.add),
                     reads=["rt1%d" % cb, "rt2%d" % cb], writes=["rt1%d" % cb])
                P.op("dve", lambda e: e.tensor_tensor(out=qT[:, h, tbs].rearrange("p (n c) -> p n c", n=4),
                                                       in0=rt1[cb].rearrange("p (n c) -> p n c", n=4),
                                                       in1=qdec[:, h, :].unsqueeze(1).to_broadcast([128, 4, 128]), op=ALU.mult),
                     reads=["rt1%d" % cb, "qdec"], writes=["qT"])
            else:
                P.op("pool", lambda e: e.tensor_tensor(out=krT[:, h, tbs], in0=rt1[cb], in1=rt2[cb], op=ALU.add),
                     reads=["rt1%d" % cb, "rt2%d" % cb], writes=["krT"])

        for u in range(len(units) + 1):
            if u < len(units):
                tb, which, h = units[u]
                if which == 0 and h == 0:
                    ada_chunk(8 + tb)
                u_s0(u)
            if u >= 1:
                u_s1(u - 1)

        if debug == "A2b":
            for h in range(4):
                dump("qT%d" % h, qT[:, h, :])
                dump("krT%d" % h, krT[:, h, :])
            return finish()
        P.barrier()
        S = cv(R3, TB, 512)
        maskT = cv(R3, TB + 512, 512, F32, "p (h c) -> p h c", h=4)
        gnw_b = cv(R3, TB + 1024, 512)
        sT = [cv(R3, TB + 1536 + i * 256, 256, BF16, "p (h c) -> p h c", h=4) for i in range(2)]
        rn = [cv(R3, TB + 2048 + i * 512, 512) for i in range(2)]
        sg = [cv(R3, TB + 3072 + i * 256, 256, BF16) for i in range(6)]
        ro = [cv(R3, TB + 4608 + i * 256, 256, BF16) for i in range(2)]
        r_sb = [cv(R3, TB + 5120 + i * 512, 512) for i in range(4)]
        P.dma("sp", lambda e: e.dma_start(out=maskT, in_=c_mask.rearrange("p (h c) -> p h c", h=4)), writes=["maskT"])
        P.dma("sp", lambda e: e.dma_start(out=gnw_b, in_=gnw.partition_broadcast(128)), writes=["gnwb"])
        for tile in range(NT):
            ts = slice(tile * 128, (tile + 1) * 128)
            bk = tile % 2
            for h in range(4):
                P.op("pe", lambda e, h=h, ts=ts, bk=bk: e.transpose(out=pbb(bk)[:, h * 128:(h + 1) * 128], in_=krT[:, h, ts], identity=identb),
                     reads=["krT", "identb"], writes=["pb%d" % bk], signal=(h == 3))
            P.op("dve", lambda e, tile=tile, bk=bk: e.tensor_tensor(out=k_tok[:, tile, :].rearrange("p (h d) -> p h d", h=4),
                                                                  in0=pbb(bk)[:, 0:512].rearrange("p (h d) -> p h d", h=4),
                                                                  in1=vdec.unsqueeze(2).to_broadcast([128, 4, 128]), op=ALU.mult),
                 reads=["pb%d" % bk, "vdec"], writes=["k_tok"])
        P.op("pool", lambda e: e.memset(S, 0.0), writes=["S"])
        def state_step(n):
            P.op("act", lambda e: e.activation(out=prev[:, n, :], in_=S, func=AF.Copy), reads=["S"], writes=["prev%d" % n])
            if n < NT - 1:
                bs = 2 + n % 2
                for h in range(4):
                    hs = slice(h * 128, (h + 1) * 128)
                    P.op("pe", lambda e, hs=hs: e.matmul(pb(bs)[:, hs], lhsT=k_tok[:, n, hs], rhs=v_tok[:, n, hs], start=True, stop=True),
                         reads=["k_tok", "v_tok"], writes=["pb%d" % bs], signal=(h == 3))
                for h in range(4):
                    hs = slice(h * 128, (h + 1) * 128)
                    P.op("dve", lambda e, h=h, hs=hs: e.scalar_tensor_tensor(out=S[:, hs], in0=S[:, hs], scalar=CD[h], in1=pb(bs)[:, hs],
                                                                         op0=ALU.mult, op1=ALU.add),
                         reads=["S", "pb%d" % bs], writes=["S"])

        state_step(0)

        def gset(n):
            base = (n % 4) * 64
            return (gst_all[:, base:base + 24], gst_all[:, base + 24:base + 32], gst_all[:, base + 32:base + 36], gst_all[:, base + 36:base + 40], "gs%d" % (n % 4))

        def a4_sc(n):
            ts = slice(n * 128, (n + 1) * 128)
            ba = 4 + n % 2
            for h in range(4):
                hs = slice(h * 128, (h + 1) * 128)
                P.op("pe", lambda e, h=h, hs=hs: e.matmul(pb(ba)[:, hs], lhsT=krT[:, h, ts], rhs=qT[:, h, ts], start=True, stop=True),
                     reads=["krT", "qT"], writes=["pb%d" % ba], signal=(h == 3))

        def a4_mask(n):
            ba = 4 + n % 2
            P.op("dve", lambda e: e.tensor_tensor(out=sT[n % 2], in0=pb(ba).rearrange("p (h c) -> p h c", h=4), in1=maskT, op=ALU.mult),
                 reads=["pb%d" % ba, "maskT"], writes=["sT%d" % (n % 2)])

        def a4_rg(n):
            ts = slice(n * 128, (n + 1) * 128)
            br_ = 6 + n % 2
            bg = n % 2
            for h in range(4):
                hs = slice(h * 128, (h + 1) * 128)
                P.op("pe", lambda e, h=h, hs=hs: e.matmul(pb(br_)[:, hs], lhsT=sT[n % 2][:, h, :], rhs=v_tok[:, n, hs], start=True, stop=False),
                     reads=["sT%d" % (n % 2), "v_tok"], writes=["pb%d" % br_], signal=False)
                P.op("pe", lambda e, h=h, hs=hs: e.matmul(pb(br_)[:, hs], lhsT=qT[:, h, ts], rhs=prev[:, n, hs], start=False, stop=True),
                     reads=["qT", "prev%d" % n], writes=["pb%d" % br_], signal=(h == 3))
            for kc in range(8):
                P.op("pe", lambda e, kc=kc: e.matmul(pb(bg), lhsT=uT[:, kc, ts], rhs=wg[:, kc, :], start=(kc == 0), stop=(kc == 7)),
                     reads=["uT", "wg"], writes=["pb%d" % bg], signal=(kc == 7))

        def a4_ev(n):
            br_ = 6 + n % 2
            bg = n % 2
            P.op("act", lambda e: e.activation(out=r_sb[n % 4], in_=pb(br_), func=AF.Copy), reads=["pb%d" % br_], writes=["rsb%d" % (n % 4)])
            P.op("act", lambda e: e.activation(out=sg[n % 6], in_=pb(bg), func=AF.Silu), reads=["pb%d" % bg], writes=["sg%d" % (n % 6)])

        def a4_st(n):
            gst, gmv, gve, grs, gk = gset(n)
            for h in range(4):
                hs = slice(h * 128, (h + 1) * 128)
                P.op("dve", lambda e, h=h, hs=hs: e.bn_stats(out=gst[:, h * 6:(h + 1) * 6], in_=r_sb[n % 4][:, hs]),
                     reads=["rsb%d" % (n % 4)], writes=[gk + "st"])
            for h in range(4):
                P.op("dve", lambda e, h=h: e.bn_aggr(out=gmv[:, h * 2:(h + 1) * 2], in_=gst[:, h * 6:(h + 1) * 6]), reads=[gk + "st"], writes=[gk + "mv"])

        def a4_rs(n):
            gst, gmv, gve, grs, gk = gset(n)
            P.op("pool", lambda e: e.tensor_scalar(out=gve, in0=gmv.rearrange("p (h two) -> p h two", two=2)[:, :, 1], scalar1=LN_EPS, scalar2=1.0, op0=ALU.add, op1=ALU.mult),
                 reads=[gk + "mv"], writes=[gk + "ve"])
            P.op("pool", lambda e: e.tensor_tensor(out=grs, in0=gve, in1=negh, op=ALU.pow), reads=[gk + "ve", "negh"], writes=[gk + "rs"])

        def a4_nm(n):
            gst, gmv, gve, grs, gk = gset(n)
            for h in range(4):
                hs = slice(h * 128, (h + 1) * 128)
                P.op("dve", lambda e, h=h, hs=hs: e.tensor_scalar(out=rn[n % 2][:, hs], in0=r_sb[n % 4][:, hs], scalar1=gmv[:, 2 * h:2 * h + 1],
                                                              scalar2=grs[:, h:h + 1], op0=ALU.subtract, op1=ALU.mult),
                     reads=["rsb%d" % (n % 4), gk + "mv", gk + "rs"], writes=["rn%d" % (n % 2)])

        def a4_gt(n):
            P.op("pool", lambda e: e.tensor_tensor(out=rn[n % 2], in0=rn[n % 2], in1=gnw_b, op=ALU.mult),
                 reads=["rn%d" % (n % 2), "gnwb"], writes=["rn%d" % (n % 2)])
            P.op("pool", lambda e: e.tensor_tensor(out=ro[n % 2], in0=rn[n % 2], in1=sg[n % 6], op=ALU.mult),
                 reads=["rn%d" % (n % 2), "sg%d" % (n % 6)], writes=["ro%d" % (n % 2)])

        def a4_tr(n):
            bt = 2 + n % 2
            for h in range(4):
                hs = slice(h * 128, (h + 1) * 128)
                P.op("pe", lambda e, h=h, hs=hs: e.transpose(out=pbb(bt)[:, hs], in_=ro[n % 2][:, hs], identity=identb),
                     reads=["ro%d" % (n % 2), "identb"], writes=["pb%d" % bt], signal=(h == 3))

        def a4_ct(n):
            ts = slice(n * 128, (n + 1) * 128)
            bt = 2 + n % 2
            P.op("act", lambda e: e.activation(out=catT[:, 0:4, ts], in_=pbb(bt)[:, 0:512].rearrange("p (h t) -> p h t", h=4), func=AF.Copy),
                 reads=["pb%d" % bt], writes=["catT"])

        a4_stages = [a4_sc, a4_mask, a4_rg, a4_ev, a4_st, a4_rs, a4_nm, a4_gt, a4_tr, a4_ct]
        for step in range(NT + len(a4_stages) - 1):
            for j in range(len(a4_stages) - 1, -1, -1):
                t_ = step - j
                if 0 <= t_ < NT:
                    a4_stages[j](t_)
            if step + 1 < NT:
                state_step(step + 1)
            if step == NT + 1:
                woutg_e = cv(R3, 0, 4096, BF16, "p (k d) -> p k d", k=8)
                P.dma("pool", lambda e: e.dma_start(out=woutg_e, in_=w_out.rearrange("(k p) d -> p k d", p=128)),
                      writes=["woutg"] + ["prev%d" % n_ for n_ in range(NT)])
            if step == NT + 5:
                P.op("dve", lambda e: e.tensor_tensor(out=woutg_e[:, 0:5, :], in0=woutg_e[:, 0:5, :], in1=gate1_b.unsqueeze(1).to_broadcast([128, 5, 1024]), op=ALU.mult),
                     reads=["woutg"], writes=["woutgA"])
                P.op("pool", lambda e: e.tensor_tensor(out=woutg_e[:, 5:8, :], in0=woutg_e[:, 5:8, :], in1=gate1_b.unsqueeze(1).to_broadcast([128, 3, 1024]), op=ALU.mult),
                     reads=["woutg"], writes=["woutgB"])

        if debug == "A4":
            for n in (0, 1, 15):
                dump("prev%d" % n, prev[:, n, :])
                dump("ktok%d" % n, k_tok[:, n, :])
            for k in range(4):
                dump("catT%d" % k, catT[:, k, :])
            return finish()
        P.barrier()
        acc = cv(R0, 0, 16384, F32, "p (n d) -> p n d", n=16)
        u2T = cv(R1, 0, 8192, BF16, "p (k t) -> p k t", k=8)
        woutg = cv(R3, 0, 4096, BF16, "p (k d) -> p k d", k=8)
        ln1w_b = cv(R3, 4096, 1024)
        ln1b_b = cv(R3, 5120, 1024)
        xin2 = [cv(R3, 7168 + i * 1024, 1024) for i in range(2)]
        xn2 = cv(R3, 9216, 1024)
        u2f = cv(R3, 10240, 1024, F32, "p (k t) -> p k t", k=8)
        dg = cv(R3, 11264, 256)
        P.dma("sp", lambda e: e.dma_start(out=ln1w_b, in_=ln1w.partition_broadcast(128)), writes=["ln1w"])
        P.dma("sp", lambda e: e.dma_start(out=ln1b_b, in_=ln1b.partition_broadcast(128)), writes=["ln1b"])

        def build_gate_b(col0, gdst):
            for kc in range(8):
                d_ = dg[:, (kc % 2) * 128:(kc % 2 + 1) * 128]
                P.op("dve", lambda e, kc=kc, d_=d_: e.tensor_scalar(out=d_, in0=ident, scalar1=adaCol[:, col0 + kc:col0 + kc + 1], scalar2=None, op0=ALU.mult),
                     reads=["ident", "adaCol"], writes=["dg%d" % (kc % 2)])
                P.op("pe", lambda e, kc=kc, d_=d_: e.matmul(pb(kc // 4)[:, (kc % 4) * 128:(kc % 4 + 1) * 128], lhsT=ones_f, rhs=d_, start=True, stop=True),
                     reads=["ones", "dg%d" % (kc % 2)], writes=["pb%d" % (kc // 4)])
            for hh in range(2):
                P.op("act", lambda e, hh=hh: e.activation(out=gdst[:, hh * 512:(hh + 1) * 512], in_=pb(hh), func=AF.Copy),
                     reads=["pb%d" % hh], writes=["gate_b"])

        xn2b = [xn2, cv(R3, 12288, 1024)]
        u2fb = [u2f, cv(R3, 13312, 1024, F32, "p (k t) -> p k t", k=8)]
        lgt_all = cv(R3, 11584, 576, F32, "p (n c) -> p n c", n=16)
        st2 = {}

        def a5_mm(tile):
            pr = tile % 2
            ts = slice(tile * 128, (tile + 1) * 128)
            xb = xin2[pr]; kx = "xin2%d" % pr
            P.dma("sp", lambda e: e.dma_start(out=xb, in_=x[ts, :]), writes=[kx])
            for hh in range(2):
                bm = pr * 2 + hh
                for kc in range(8):
                    P.op("pe", lambda e, kc=kc, bm=bm, hh=hh: e.matmul(pb(bm), lhsT=catT[:, kc, ts], rhs=woutg[:, kc, hh * 512:(hh + 1) * 512],
                                                                    start=(kc == 0), stop=(kc == 7)),
                         reads=["catT", "woutg", "woutgA", "woutgB"], writes=["pb%d" % bm], signal=(kc == 7))

        def a5_res(tile):
            pr = tile % 2
            xb = xin2[pr]; kx = "xin2%d" % pr
            at = acc[:, tile, :]; ka = "acc%d" % tile
            for hh in range(2):
                bm = pr * 2 + hh
                P.op("dve", lambda e, bm=bm, hh=hh: e.scalar_tensor_tensor(out=at[:, hh * 512:(hh + 1) * 512], in0=xb[:, hh * 512:(hh + 1) * 512],
                                                                        scalar=ALPHA, in1=pb(bm), op0=ALU.mult, op1=ALU.add),
                     reads=[kx, "pb%d" % bm], writes=[ka])
            st2[("ln1", tile)] = ln_stats(at, ka, pr)

        def a5_ln1(tile):
            pr = tile % 2
            at = acc[:, tile, :]; ka = "acc%d" % tile
            mean, rstd, sk = st2[("ln1", tile)]
            P.op("dve", lambda e: e.scalar_tensor_tensor(out=at, in0=at, scalar=mean, in1=ln1w_b, op0=ALU.subtract, op1=ALU.mult),
                 reads=[ka, "ln1w"] + sk, writes=[ka])
            P.op("dve", lambda e: e.scalar_tensor_tensor(out=at, in0=at, scalar=rstd, in1=ln1b_b, op0=ALU.mult, op1=ALU.add),
                 reads=[ka, "ln1b"] + sk, writes=[ka])

        def a5_sq(tile):
            pr = tile % 2
            at = acc[:, tile, :]; ka = "acc%d" % tile
            st2[("ln2", tile)] = act_stats(at, ka, 2 + pr, gate1_b, "gate_b", alpha=ALPHA)

        def a5_xn(tile):
            pr = tile % 2
            at = acc[:, tile, :]; ka = "acc%d" % tile
            xq = xn2b[pr]; kxn = "xn2%d" % pr
            scl, nb, sk2 = st2[("ln2", tile)]
            P.op("act", lambda e: e.activation(out=xq, in_=at, func=AF.Identity, scale=scl, bias=nb), reads=[ka] + sk2, writes=[kxn])

        def a5_tr(tile):
            pr = tile % 2
            xq = xn2b[pr]; kxn = "xn2%d" % pr
            for kc in range(8):
                bu = 4 + kc // 4
                P.op("pe", lambda e, kc=kc, bu=bu: e.transpose(out=pb(bu)[:, (kc % 4) * 128:(kc % 4 + 1) * 128], in_=xq[:, kc * 128:(kc + 1) * 128], identity=ident),
                     reads=[kxn, "ident"], writes=["pb%d" % bu], signal=(kc % 4 == 3))

        def a5_ev(tile):
            pr = tile % 2
            uf = u2fb[pr]; kuf = "u2f%d" % pr
            for kc in range(8):
                bu = 4 + kc // 4
                P.op("act", lambda e, kc=kc, bu=bu: e.activation(out=uf[:, kc, :], in_=pb(bu)[:, (kc % 4) * 128:(kc % 4 + 1) * 128], func=AF.Identity,
                                                               scale=adaCol[:, 32 + kc:33 + kc], bias=adaCol[:, 24 + kc:25 + kc]),
                     reads=["pb%d" % bu, "adaCol"], writes=[kuf])

        def a5_rt(tile):
            pr = tile % 2
            ts = slice(tile * 128, (tile + 1) * 128)
            uf = u2fb[pr]; kuf = "u2f%d" % pr
            P.op("pool", lambda e: e.tensor_copy(out=u2T[:, :, ts], in_=uf), reads=[kuf], writes=["u2T"])
            bl = 6 + pr
            for kc in range(8):
                P.op("pe", lambda e, kc=kc: e.matmul(pb(bl)[:, 0:36], lhsT=uf[:, kc, :], rhs=wr_sb[:, kc, :], start=(kc == 0), stop=(kc == 7)),
                     reads=[kuf, "wr"], writes=["pb%d" % bl], signal=(kc == 7))

        def a5_lg(tile):
            bl = 6 + tile % 2
            P.op("dve", lambda e: e.tensor_tensor(out=lgt_all[:, tile, :], in0=pb(bl)[:, 0:36], in1=br_b, op=ALU.add),
                 reads=["pb%d" % bl, "brb"], writes=["lgt"])

        a5_stages = [a5_mm, a5_res, a5_ln1, a5_sq, a5_xn, a5_tr, a5_ev, a5_rt, a5_lg]
        w13 = [cv(R2, i * 2048, 2048, BF16, "p (a k f) -> p a k f", a=2, k=8) for i in range(4)]
        w2s = [cv(R3, i * 1024, 1024, BF16, "p (c d) -> p c d", c=2) for i in range(4)]

        loadq = []

        def load_expert(ex, slot, extra=()):
            ex_ = list(extra)
            loadq.append(lambda: P.dma("pool", lambda e: e.dma_start(out=w13[slot][:, 0], in_=w1[ex].rearrange("(k p) f -> p k f", p=128)),
                                       writes=["w13_%d" % slot] + ex_))
            loadq.append(lambda: P.dma("pool", lambda e: e.dma_start(out=w13[slot][:, 1], in_=w3[ex].rearrange("(k p) f -> p k f", p=128)),
                                       writes=["w13_%d" % slot] + ex_))
            loadq.append(lambda: P.dma("pool", lambda e: e.dma_start(out=w2s[slot], in_=w2[ex].rearrange("(c p) d -> p c d", p=128)),
                                       writes=["w2_%d" % slot] + ex_))

        def pump(n):
            for _ in range(n):
                if loadq:
                    loadq.pop(0)()

        for step in range(NT + len(a5_stages) - 1):
            for j in range(len(a5_stages) - 1, -1, -1):
                t_ = step - j
                if 0 <= t_ < NT:
                    a5_stages[j](t_)
            if step == NT:
                for i in range(EG):
                    load_expert(i, i, extra=["catT", "woutg", "woutgA", "woutgB"])
            if step >= NT:
                pump(1)

        pump(len(loadq))
        RB = 7168
        def rc(off, ncol):
            return cv(R3, RB + off, ncol)
        gmax = rc(0, 16); gmask = rc(16, 64); sh4 = rc(80, 64); gsum = rc(144, 16); gprob = rc(160, 16); pen = rc(176, 64)
        v1 = rc(240, 16); v2 = rc(256, 16); dd = rc(272, 16); ed = rc(288, 16); p1 = rc(304, 16); p2 = rc(320, 16)
        elm = rc(512, 512); m1 = rc(1024, 512); m2 = rc(1536, 512); gts_all = rc(2048, 512); tmpg = rc(2560, 512)
        K_ = "rtb"
        L4 = lgt_all[:, :, 32:36]
        g3 = lambda ap_, w: ap_.rearrange("p (n c) -> p n c", n=16) if w else ap_
        P.op("dve", lambda e: e.tensor_reduce(out=gmax, in_=L4, axis=AX.X, op=ALU.max), reads=["lgt"], writes=[K_, "xin20", "xin21", "xn20"])
        P.op("dve", lambda e: e.tensor_tensor(out=g3(gmask, 1), in0=L4, in1=gmax.unsqueeze(2).to_broadcast([128, 16, 4]), op=ALU.is_ge), reads=["lgt", K_], writes=[K_])
        P.op("dve", lambda e: e.tensor_tensor(out=g3(sh4, 1), in0=L4, in1=gmax.unsqueeze(2).to_broadcast([128, 16, 4]), op=ALU.subtract), reads=["lgt", K_], writes=[K_])
        P.op("act", lambda e: e.activation(out=sh4, in_=sh4, func=AF.Exp), reads=[K_], writes=[K_])
        P.op("dve", lambda e: e.tensor_reduce(out=gsum, in_=g3(sh4, 1), axis=AX.X, op=ALU.add), reads=[K_], writes=[K_])
        P.op("dve", lambda e: e.reciprocal(out=gprob, in_=gsum), reads=[K_], writes=[K_])
        P.op("dve", lambda e: e.tensor_scalar(out=pen, in0=gmask, scalar1=1.0, scalar2=BIG, op0=ALU.subtract, op1=ALU.mult), reads=[K_], writes=[K_])
        P.op("dve", lambda e: e.tensor_tensor(out=elm.rearrange("p (n g i) -> p n g i", n=16, g=4),
                                              in0=lgt_all[:, :, 0:32].rearrange("p n (g i) -> p n g i", g=4),
                                              in1=g3(pen, 1).unsqueeze(3).to_broadcast([128, 16, 4, 8]), op=ALU.add), reads=["lgt", K_], writes=[K_])
        P.op("dve", lambda e: e.tensor_reduce(out=v1, in_=g3(elm, 1), axis=AX.X, op=ALU.max), reads=[K_], writes=[K_])
        P.op("dve", lambda e: e.tensor_tensor(out=g3(m1, 1), in0=g3(elm, 1), in1=v1.unsqueeze(2).to_broadcast([128, 16, 32]), op=ALU.is_ge), reads=[K_], writes=[K_])
        P.op("dve", lambda e: e.scalar_tensor_tensor(out=elm, in0=m1, scalar=-BIG, in1=elm, op0=ALU.mult, op1=ALU.add), reads=[K_], writes=[K_])
        P.op("dve", lambda e: e.tensor_reduce(out=v2, in_=g3(elm, 1), axis=AX.X, op=ALU.max), reads=[K_], writes=[K_])
        P.op("dve", lambda e: e.tensor_tensor(out=g3(m2, 1), in0=g3(elm, 1), in1=v2.unsqueeze(2).to_broadcast([128, 16, 32]), op=ALU.is_ge), reads=[K_], writes=[K_])
        P.op("dve", lambda e: e.tensor_tensor(out=dd, in0=v2, in1=v1, op=ALU.subtract), reads=[K_], writes=[K_])
        P.op("act", lambda e: e.activation(out=ed, in_=dd, func=AF.Exp), reads=[K_], writes=[K_])
        P.op("dve", lambda e: e.tensor_scalar_add(out=p1, in0=ed, scalar1=1.0), reads=[K_], writes=[K_])
        P.op("dve", lambda e: e.reciprocal(out=p1, in_=p1), reads=[K_], writes=[K_])
        P.op("dve", lambda e: e.tensor_tensor(out=p2, in0=ed, in1=p1, op=ALU.mult), reads=[K_], writes=[K_])
        P.op("dve", lambda e: e.tensor_tensor(out=p1, in0=p1, in1=gprob, op=ALU.mult), reads=[K_], writes=[K_])
        P.op("dve", lambda e: e.tensor_tensor(out=p2, in0=p2, in1=gprob, op=ALU.mult), reads=[K_], writes=[K_])
        P.op("dve", lambda e: e.tensor_tensor(out=g3(gts_all, 1), in0=g3(m1, 1), in1=p1.unsqueeze(2).to_broadcast([128, 16, 32]), op=ALU.mult), reads=[K_], writes=[K_])
        P.op("dve", lambda e: e.tensor_tensor(out=g3(tmpg, 1), in0=g3(m2, 1), in1=p2.unsqueeze(2).to_broadcast([128, 16, 32]), op=ALU.mult), reads=[K_], writes=[K_])
        P.op("dve", lambda e: e.tensor_tensor(out=gts_all, in0=gts_all, in1=tmpg, op=ALU.add), reads=[K_], writes=[K_])
        for q4 in range(4):
            for i in range(4):
                n = q4 * 4 + i
                P.op("pe", lambda e, n=n, q4=q4, i=i: e.transpose(out=pb(q4)[0:32, i * 128:(i + 1) * 128], in_=gts_all[:, n * 32:(n + 1) * 32], identity=ident),
                     reads=[K_, "ident"], writes=["pb%d" % q4], signal=(i == 3))
            P.op("act", lambda e, q4=q4: e.activation(out=gatesT[0:32, q4 * 512:(q4 + 1) * 512], in_=pb(q4)[0:32, :], func=AF.Copy),
                 reads=["pb%d" % q4], writes=["gatesT"])
        P.dma("sp", lambda e: e.dma_start(out=gscr, in_=gatesT[0:32, :]), reads=["gatesT"], writes=["gscr"])

        if debug == "A5":
            for n in (0, 7, 15):
                dump("acc%d" % n, acc[:, n, :])
            for k in (0, 7):
                dump("u2T%d" % k, u2T[:, k, :])
            dump("gatesT", gatesT[0:32, :])
            return finish()
        P.barrier()
        hT = [cv(R3, 4096 + i * 1024, 1024, BF16, "p (e c t) -> p e c t", e=2, c=2) for i in range(2)]
        s_t = [cv(R3, 6144 + i * 512, 512) for i in range(2)]
        t_t = [cv(R3, 7168 + i * 256, 256, BF16) for i in range(2)]
        gsb = [cv(R3, 7680 + i * 256, 256, BF16) for i in range(2)]
        gate2_b = cv(R3, 8192, 1024)
        gB = [cv(R3, 9216 + i * 256, 256, BF16) for i in range(2 * EG)]
        build_gate_b(40, gate2_b)
        ln2w_b = cv(R3, 11264, 1024)
        ln2b_b = cv(R3, 12288, 1024)
        otb = [cv(R3, 13312, 1024), gate2_b]
        otk = ["ot0", "gate_b"]
        P.dma("sp", lambda e: e.dma_start(out=ln2w_b, in_=ln2w.partition_broadcast(128)), writes=["ln2w", "dg0", "dg1"])
        P.dma("sp", lambda e: e.dma_start(out=ln2b_b, in_=ln2b.partition_broadcast(128)), writes=["ln2b"])

        fin_st = {}

        def fin_a(tile):
            at = acc[:, tile, :]
            o = otb[tile % 2]
            ko = otk[tile % 2]
            fin_st[tile] = act_stats(at, "acc%d" % tile, 4 + tile % 2, o, ko)

        def fin_b(tile):
            at = acc[:, tile, :]
            ka = "acc%d" % tile
            o = otb[tile % 2]
            ko = otk[tile % 2]
            rstd, nb, sk = fin_st[tile]
            P.op("act", lambda e: e.activation(out=o, in_=at, func=AF.Identity, scale=rstd, bias=nb), reads=[ka] + sk, writes=[ko])
            P.op("dve", lambda e: e.tensor_tensor(out=o, in0=o, in1=ln2w_b, op=ALU.mult), reads=[ko, "ln2w"], writes=[ko])
            P.op("dve", lambda e: e.tensor_tensor(out=o, in0=o, in1=ln2b_b, op=ALU.add), reads=[ko, "ln2b"], writes=[ko])
            P.dma("sp", lambda e: e.dma_start(out=out[tile * 128:(tile + 1) * 128, :], in_=o), reads=[ko])

        def finalize_tile(tile):
            if tile >= 1:
                fin_b(tile - 1)
            fin_a(tile)


        def issue_gates(u):
            G_, tb_ = u // 4, u % 4
            for ei_ in range(EG):
                ex_ = G_ * EG + ei_
                sl_ = ei_ * 2 + u % 2
                P.dma("sp", lambda e, ex_=ex_, sl_=sl_, tb_=tb_: e.dma_start(out=gB[sl_], in_=gscr[ex_:ex_ + 1, tb_ * 512:(tb_ + 1) * 512].partition_broadcast(128)),
                      writes=["gB%d" % sl_])

        def scale_w2(slot):
            P.op("pool", lambda e: e.tensor_tensor(out=w2s[slot], in0=w2s[slot], in1=gate2_b.unsqueeze(1).to_broadcast([128, 2, 1024]), op=ALU.mult),
                 reads=["w2_%d" % slot, "gate_b"], writes=["w2_%d" % slot])

        NG = NE // EG
        for i in range(EG):
            load_expert(EG + i, EG + i)
        ucount = 0
        hcount = 0
        ycount = 0

        def stage2(G, tb, par, per_tile=None, inter=None):
            nonlocal ycount
            for ti in range(4):
                tile = tb * 4 + ti
                if per_tile is not None and ti >= 1:
                    per_tile(tile - 1)
                if inter is not None and ti >= 1 and inter:
                    inter.pop(0)()
                for hh in range(2):
                    by = 6 + ycount % 2
                    ycount += 1
                    idx = 0
                    for ei in range(EG):
                        slot = (G % 2) * EG + ei
                        for fc in range(2):
                            P.op("pe", lambda e, par=par, ei=ei, fc=fc, ti=ti, slot=slot, hh=hh, by=by, idx=idx: e.matmul(
                                pb(by), lhsT=hT[par][:, ei, fc, ti * 128:(ti + 1) * 128], rhs=w2s[slot][:, fc, hh * 512:(hh + 1) * 512],
                                start=(idx == 0), stop=(idx == 2 * EG - 1)),
                                reads=["hT%d" % par, "w2_%d" % slot], writes=["pb%d" % by], signal=(idx == 2 * EG - 1))
                            idx += 1
                    P.op("dve", lambda e, tile=tile, hh=hh, by=by: e.tensor_tensor(out=acc[:, tile, hh * 512:(hh + 1) * 512],
                                                                                 in0=acc[:, tile, hh * 512:(hh + 1) * 512], in1=pb(by), op=ALU.add),
                         reads=["pb%d" % by, "acc%d" % tile], writes=["acc%d" % tile])

        pending = None
        issue_gates(0)
        for G in range(NG):
            for tb in range(4):
                tbs = slice(tb * 512, (tb + 1) * 512)
                par = ucount % 2
                if ucount + 1 < NG * 4:
                    issue_gates(ucount + 1)
                ucount += 1
                if tb == 0:
                    for i in range(EG):
                        scale_w2((G % 2) * EG + i)
                s1 = []
                for ei in range(EG):
                  for fc in range(2):
                    s1.append((ei, fc))

                def sub_unit(ei, fc, G=G, tbs=tbs, par=par, uc=ucount):
                    nonlocal hcount
                    ex = G * EG + ei
                    slot = (G % 2) * EG + ei
                    bgt = 4 + (uc * EG + ei) % 2
                    gi = (uc * EG + ei) % 2
                    if True:
                        fs = slice(fc * 128, (fc + 1) * 128)
                        hb = hcount % 2
                        hcount += 1
                        for a in range(2):
                            bh = a * 2 + hb
                            for kc in range(8):
                                P.op("pe", lambda e, a=a, kc=kc, fs=fs, slot=slot, tbs=tbs, bh=bh: e.matmul(pb(bh), lhsT=w13[slot][:, a, kc, fs], rhs=u2T[:, kc, tbs],
                                                                                                    start=(kc == 0), stop=(kc == 7)),
                                     reads=["w13_%d" % slot, "u2T"], writes=["pb%d" % bh], signal=(kc == 7))
                        P.op("act", lambda e, hb=hb: e.activation(out=s_t[hb], in_=pb(hb), func=AF.Silu), reads=["pb%d" % hb], writes=["s_t%d" % hb])
                        P.op("dve", lambda e, hb=hb: e.tensor_tensor(out=t_t[hb], in0=pb(2 + hb), in1=s_t[hb], op=ALU.mult),
                             reads=["pb%d" % (2 + hb), "s_t%d" % hb], writes=["t_t%d" % hb])
                        P.op("pool", lambda e, hb=hb, par=par, ei=ei, fc=fc: e.tensor_tensor(out=hT[par][:, ei, fc, :], in0=t_t[hb], in1=gB[ei * 2 + par], op=ALU.mult),
                             reads=["t_t%d" % hb, "gB%d" % (ei * 2 + par)], writes=["hT%d" % par])
                thunks = [(lambda ei=ei, fc=fc: sub_unit(ei, fc)) for ei, fc in s1]
                last_grp = pending is not None and pending[0] == NG - 1 and debug != "B"
                if not last_grp:
                    for th in thunks:
                        th()
                    thunks = []
                else:
                    thunks.pop(0)()
                if pending is not None:
                    pG, ptb, _ = pending
                    fin = finalize_tile if (pG == NG - 1 and debug != "B") else None
                    stage2(*pending, per_tile=fin, inter=thunks)
                    while thunks:
                        thunks.pop(0)()
                    if fin is not None:
                        finalize_tile(ptb * 4 + 3)
                    if ptb == 3 and pG + 2 < NG:
                        for i in range(EG):
                            load_expert((pG + 2) * EG + i, (pG % 2) * EG + i)
                pump(2)
                pending = (G, tb, par)
        stage2(*pending, per_tile=(finalize_tile if debug != "B" else None))
        if debug != "B":
            finalize_tile(pending[1] * 4 + 3)
            fin_b(NT - 1)

        if debug == "B":
            for n in (0, 7, 15):
                dump("acc%d" % n, acc[:, n, :])
            return finish()
        P.barrier()
        P.wait_all("sp")
        P.emit(nc, st)
    return nc, consts


_CACHE = {}


def kernel(x, c, positions, w_ada, b_ada, w_in, ret_gn_w, w_pool, pool_scale, w_out,
           ln1_w, ln1_b, w_group, b_group, w_router, b_router, w1, w3, w2, ln2_w, ln2_b):
    if "nc" not in _CACHE:
        _CACHE["nc"] = build_nc()
    nc, consts = _CACHE["nc"]
    f = lambda a: np.ascontiguousarray(np.asarray(a, dtype=np.float32))
    x = f(x); c = f(c)
    positions = np.ascontiguousarray(np.asarray(positions, dtype=np.int32))
    shared = dict(
        w_ada=f(w_ada[0]), b_ada=f(b_ada[0])[None, :], w_in=f(w_in[0]), gnw=f(ret_gn_w[0])[None, :],
        w_pool=f(w_pool[0]), pscale=f(np.asarray(pool_scale[0]).reshape(4, 128).T), w_out=f(w_out[0]),
        ln1w=f(ln1_w[0])[None, :], ln1b=f(ln1_b[0])[None, :], ln2w=f(ln2_w[0])[None, :], ln2b=f(ln2_b[0])[None, :],
        wr=f(np.concatenate([np.asarray(w_router[0]), np.asarray(w_group[0])], axis=1)),
        br=f(np.concatenate([np.asarray(b_router[0]), np.asarray(b_group[0])]))[None, :],
        w1=f(np.asarray(w1[0]).reshape(NE, D, 256)), w3=f(np.asarray(w3[0]).reshape(NE, D, 256)),
        w2=f(np.asarray(w2[0]).reshape(NE, 256, D)),
        **consts,
    )
    in_maps = []
    for b in range(8):
        m = dict(shared)
        m["x"] = x[b]
        m["c_col"] = f(c[b].reshape(8, 128).T)
        m["pos"] = positions[b][None, :]
        in_maps.append(m)
    res = run_bass_kernel_spmd(nc, in_maps, core_ids=list(range(8)))
    return np.stack([np.asarray(r["out"], dtype=np.float32) for r in res.results], axis=0)
```
